# Optimizing a Trainium2 kernel written in Bass

```python
import math
import jax, jax.numpy as jnp
from jax import lax
import numpy as np

D_MODEL = 1024
BATCH = 8
SEQ = 2048
DEPTH = 2

GRID_W = 64
CTX_LEN = 256
D_HYENA = D_MODEL
D_LRU = D_MODEL
D_SC = D_MODEL
HYENA_ORDER = 2
HYENA_CONV_W = 3
HYENA_EMB_DIM = 33
HYENA_FILTER_W = 64
HYENA_FAST_DECAY = 0.3
HYENA_SLOW_DECAY = 1.5
HYENA_TARGET = 1e-2
HYENA_MAX_DECAY = math.log(HYENA_TARGET) / HYENA_FAST_DECAY
HYENA_MIN_DECAY = math.log(HYENA_TARGET) / HYENA_SLOW_DECAY
LRU_HEADS = 16
LRU_BLOCK = D_LRU // LRU_HEADS
LRU_CONV_W = 4
LRU_C = 8.0
SC_CONV_W = 3
N_EXPERTS = 16
EC_CAPACITY = 2
D_EXPERT = 2816
NORM_EPS = 1e-6
OFF_LRU_GATE = 3 * D_HYENA
OFF_LRU_REC = OFF_LRU_GATE + D_LRU
OFF_SC = OFF_LRU_REC + D_LRU
OFF_GATE = OFF_SC + 3 * D_SC
D_IN_PROJ = OFF_GATE + 3 * D_MODEL

kernel_name = 'hybrid_hyena_rglru_shortconv_ecmoe_dit'


def rmsnorm(x, g):
    xf = x.astype(jnp.float32)
    y = xf * lax.rsqrt(jnp.mean(xf * xf, axis=-1, keepdims=True) + NORM_EPS)
    return (y * g.astype(jnp.float32)).astype(x.dtype)


def modulate(x, shift, scale):
    return x * (1.0 + scale) + shift


def dwconv(x, w, b, left):
    K = w.shape[0]
    L = x.shape[1]
    xp = jnp.pad(x, ((0, 0), (left, K - 1 - left), (0, 0)))
    y = sum(xp[:, k:k + L] * w[k] for k in range(K))
    return y if b is None else y + b


def sincos_1d(pos, dim):
    half = dim // 2
    omega = 1.0 / (10000.0 ** (jnp.arange(half, dtype=jnp.float32) / half))
    ang = pos[:, None] * omega[None, :]
    return jnp.concatenate([jnp.sin(ang), jnp.cos(ang)], axis=-1)


def grid_pos_embed(rows):
    half = D_MODEL // 2
    emb_r = sincos_1d(jnp.arange(rows, dtype=jnp.float32), half)
    emb_c = sincos_1d(jnp.arange(GRID_W, dtype=jnp.float32), half)
    emb = jnp.concatenate([jnp.broadcast_to(emb_r[:, None, :], (rows, GRID_W, half)),
                           jnp.broadcast_to(emb_c[None, :, :], (rows, GRID_W, half))], axis=-1)
    return emb.reshape(rows * GRID_W, D_MODEL)


def hyena_frequency_response(L, w1, b1, w2, b2, w3, freq):
    f32 = jnp.float32
    bands = (HYENA_EMB_DIM - 1) // 2
    t01 = jnp.linspace(0.0, 1.0, L, dtype=f32)[:, None]
    w = (2.0 * math.pi / L) * jnp.arange(L, dtype=f32)[:, None]
    f = jnp.linspace(1e-4, bands - 1, bands, dtype=f32)[None, :]
    feats = jnp.concatenate([t01, jnp.cos(f * w), -jnp.sin(f * w)], axis=-1)
    h = jnp.sin(freq * (feats @ w1 + b1))
    h = jnp.sin(freq * (h @ w2 + b2))
    h = (h @ w3).astype(f32).reshape(L, HYENA_ORDER, 2, D_HYENA)
    deltas = jnp.linspace(HYENA_MIN_DECAY, HYENA_MAX_DECAY, D_HYENA, dtype=f32)
    h = h * jnp.exp(-t01 * jnp.abs(deltas))[:, None, None, :]
    fwd = h[:, :, 0]
    bwd = h[:0:-1, :, 1]
    buf = jnp.concatenate([fwd, jnp.zeros_like(fwd[:1]), bwd], axis=0)
    buf = buf / jnp.sum(jnp.abs(buf), axis=0, keepdims=True)
    return jnp.fft.rfft(buf, axis=0)


def fft_conv(u, kf, bias):
    L = u.shape[1]
    uf32 = u.astype(jnp.float32)
    uf = jnp.fft.rfft(uf32, n=2 * L, axis=1)
    y = jnp.fft.irfft(uf * kf[None], n=2 * L, axis=1)[:, :L]
    return (y + uf32 * bias.astype(jnp.float32)).astype(u.dtype)


def hyena_mixer(proj, conv_w, conv_b, kf, bias):
    u = dwconv(proj, conv_w, conv_b, HYENA_CONV_W // 2)
    x1, x2, v = jnp.split(u, 3, axis=-1)
    z = x1 * fft_conv(v, kf[:, 0], bias[0])
    return x2 * fft_conv(z, kf[:, 1], bias[1])


def _linear_combine(p, q):
    a1, b1 = p
    a2, b2 = q
    return a1 * a2, a2 * b1 + b2


def rglru(xc, wa, ba, wx, bx, lam, h0, reverse):
    B, L, _ = xc.shape
    xf = xc.astype(jnp.float32)
    xb = xf.reshape(B, L, LRU_HEADS, LRU_BLOCK)
    r = jax.nn.sigmoid(jnp.einsum('blhi,hij->blhj', xb, wa).reshape(B, L, D_LRU) + ba)
    i = jax.nn.sigmoid(jnp.einsum('blhi,hij->blhj', xb, wx).reshape(B, L, D_LRU) + bx)
    log_a = -LRU_C * r * jax.nn.softplus(-lam.astype(jnp.float32))
    a = jnp.exp(log_a)
    b = jnp.sqrt(-jnp.expm1(2.0 * log_a)) * (i * xf)
    first = L - 1 if reverse else 0
    b = b.at[:, first].add(a[:, first] * h0.astype(jnp.float32))
    _, h = lax.associative_scan(_linear_combine, (a, b), reverse=reverse, axis=1)
    return h.astype(xc.dtype)


def lru_scans(rec_in, p, h0_f, h0_b):
    xc = dwconv(rec_in, p['lru_conv_w'], p['lru_conv_b'], LRU_CONV_W // 2)
    hf = rglru(xc, p['lru_wa'][0], p['lru_ba'][0], p['lru_wx'][0], p['lru_bx'][0], p['lru_lambda'][0], h0_f, False)
    hb = rglru(xc, p['lru_wa'][1], p['lru_ba'][1], p['lru_wx'][1], p['lru_bx'][1], p['lru_lambda'][1], h0_b, True)
    return hf, hb


def shortconv_mixer(proj, conv_w):
    bg, cg, xv = jnp.split(proj, 3, axis=-1)
    return bg * dwconv(cg * xv, conv_w, None, SC_CONV_W // 2)


def mixer_sublayer(h, p, h0_f, h0_b):
    L = h.shape[1]
    proj = h @ p['w_in']
    hy_in, lru_gate, lru_rec, sc_in, gate_logits = jnp.split(proj, [OFF_LRU_GATE, OFF_LRU_REC, OFF_SC, OFF_GATE], axis=-1)
    kf = hyena_frequency_response(L, p['hy_filt_w1'], p['hy_filt_b1'], p['hy_filt_w2'], p['hy_filt_b2'], p['hy_filt_w3'], p['hy_filt_freq'])
    y_a = hyena_mixer(hy_in, p['hy_conv_w'], p['hy_conv_b'], kf, p['hy_bias'])
    hf, hb = lru_scans(lru_rec, p, h0_f, h0_b)
    y_b = jax.nn.gelu(lru_gate) * (hf + hb)
    y_c = shortconv_mixer(sc_in, p['sc_conv_w'])
    g_a, g_b, g_c = jnp.split(jax.nn.sigmoid(gate_logits), 3, axis=-1)
    merged = g_a * (y_a @ p['w_hy_out']) + g_b * (y_b @ p['w_lru_out']) + g_c * (y_c @ p['w_sc_out'])
    return merged @ p['w_o'], hf[:, -1], hb[:, 0]


def ec_moe(h, w_router, wg, wu, wd):
    n, d = h.shape[1], h.shape[2]
    cap = EC_CAPACITY * n // N_EXPERTS

    def per_set(hs):
        aff = jax.nn.softmax((hs @ w_router).astype(jnp.float32), axis=-1)
        gate, idx = lax.top_k(aff.T, cap)
        xs = hs[idx]
        hid = jax.nn.silu(jnp.einsum('ecd,edf->ecf', xs, wg)) * jnp.einsum('ecd,edf->ecf', xs, wu)
        ys = jnp.einsum('ecf,efd->ecd', hid, wd) * gate[..., None].astype(hs.dtype)
        return jnp.zeros_like(hs).at[idx.reshape(-1)].add(ys.reshape(-1, d))

    return jax.vmap(per_set)(h)


def setup_inputs(seed: int = 0) -> dict:
    key = jax.random.key(seed)
    ks = iter(jax.random.split(key, 48))
    f32 = jnp.float32

    def nrm(shape, scale):
        return scale * jax.random.normal(next(ks), shape, f32)

    D = D_MODEL
    a0 = jax.random.uniform(next(ks), (DEPTH, 2, D_LRU), f32, 0.9, 0.999) ** (1.0 / LRU_C)
    lru_lambda = jnp.log(a0) - jnp.log1p(-a0)
    return {
        'x': nrm((BATCH, SEQ, D), 1.0),
        'c': nrm((BATCH, D), 1.0),
        'ctx': nrm((BATCH, CTX_LEN, D), 1.0),
        'c_ctx': nrm((D,), 1.0),
        'w_ada': nrm((DEPTH, D, 6 * D), 0.02),
        'b_ada': nrm((DEPTH, 6 * D), 0.02),
        'norm1_g': 1.0 + nrm((DEPTH, D), 0.05),
        'norm2_g': 1.0 + nrm((DEPTH, D), 0.05),
        'w_in': nrm((DEPTH, D, D_IN_PROJ), D ** -0.5),
        'hy_conv_w': nrm((DEPTH, HYENA_CONV_W, 3 * D_HYENA), HYENA_CONV_W ** -0.5),
        'hy_conv_b': nrm((DEPTH, 3 * D_HYENA), 0.02),
        'hy_filt_w1': nrm((DEPTH, HYENA_EMB_DIM, HYENA_FILTER_W), HYENA_EMB_DIM ** -0.5),
        'hy_filt_b1': nrm((DEPTH, HYENA_FILTER_W), 0.1),
        'hy_filt_w2': nrm((DEPTH, HYENA_FILTER_W, HYENA_FILTER_W), HYENA_FILTER_W ** -0.5),
        'hy_filt_b2': nrm((DEPTH, HYENA_FILTER_W), 0.1),
        'hy_filt_w3': nrm((DEPTH, HYENA_FILTER_W, HYENA_ORDER * 2 * D_HYENA), HYENA_FILTER_W ** -0.5),
        'hy_filt_freq': 1.0 + nrm((DEPTH, HYENA_FILTER_W), 0.05),
        'hy_bias': nrm((DEPTH, HYENA_ORDER, D_HYENA), 1.0),
        'lru_conv_w': nrm((DEPTH, LRU_CONV_W, D_LRU), LRU_CONV_W ** -0.5),
        'lru_conv_b': nrm((DEPTH, D_LRU), 0.02),
        'lru_wa': nrm((DEPTH, 2, LRU_HEADS, LRU_BLOCK, LRU_BLOCK), LRU_BLOCK ** -0.5),
        'lru_ba': nrm((DEPTH, 2, D_LRU), 0.02),
        'lru_wx': nrm((DEPTH, 2, LRU_HEADS, LRU_BLOCK, LRU_BLOCK), LRU_BLOCK ** -0.5),
        'lru_bx': nrm((DEPTH, 2, D_LRU), 0.02),
        'lru_lambda': lru_lambda,
        'sc_conv_w': nrm((DEPTH, SC_CONV_W, D_SC), SC_CONV_W ** -0.5),
        'w_hy_out': nrm((DEPTH, D_HYENA, D), D_HYENA ** -0.5),
        'w_lru_out': nrm((DEPTH, D_LRU, D), D_LRU ** -0.5),
        'w_sc_out': nrm((DEPTH, D_SC, D), D_SC ** -0.5),
        'w_o': nrm((DEPTH, D, D), D ** -0.5),
        'w_router': nrm((DEPTH, D, N_EXPERTS), D ** -0.5),
        'w_exp_gate': nrm((DEPTH, N_EXPERTS, D, D_EXPERT), D ** -0.5),
        'w_exp_up': nrm((DEPTH, N_EXPERTS, D, D_EXPERT), D ** -0.5),
        'w_exp_down': nrm((DEPTH, N_EXPERTS, D_EXPERT, D), D_EXPERT ** -0.5),
        'final_norm_g': 1.0 + nrm((D,), 0.05),
    }


def reference(x, c, ctx, c_ctx, w_ada, b_ada, norm1_g, norm2_g, w_in, hy_conv_w, hy_conv_b,
              hy_filt_w1, hy_filt_b1, hy_filt_w2, hy_filt_b2, hy_filt_w3, hy_filt_freq, hy_bias,
              lru_conv_w, lru_conv_b, lru_wa, lru_ba, lru_wx, lru_bx, lru_lambda, sc_conv_w,
              w_hy_out, w_lru_out, w_sc_out, w_o, w_router, w_exp_gate, w_exp_up, w_exp_down,
              final_norm_g):
    B, n_lat, _ = x.shape
    ROWS = n_lat // GRID_W
    x = x + grid_pos_embed(ROWS).astype(x.dtype)[None]
    xc = ctx
    zeros_state = jnp.zeros((B, D_LRU), jnp.float32)
    for l in range(DEPTH):
        last = l == DEPTH - 1
        p = {
            'w_in': w_in[l], 'hy_conv_w': hy_conv_w[l], 'hy_conv_b': hy_conv_b[l],
            'hy_filt_w1': hy_filt_w1[l], 'hy_filt_b1': hy_filt_b1[l], 'hy_filt_w2': hy_filt_w2[l],
            'hy_filt_b2': hy_filt_b2[l], 'hy_filt_w3': hy_filt_w3[l], 'hy_filt_freq': hy_filt_freq[l],
            'hy_bias': hy_bias[l], 'lru_conv_w': lru_conv_w[l], 'lru_conv_b': lru_conv_b[l],
            'lru_wa': lru_wa[l], 'lru_ba': lru_ba[l], 'lru_wx': lru_wx[l], 'lru_bx': lru_bx[l],
            'lru_lambda': lru_lambda[l], 'sc_conv_w': sc_conv_w[l], 'w_hy_out': w_hy_out[l],
            'w_lru_out': w_lru_out[l], 'w_sc_out': w_sc_out[l], 'w_o': w_o[l],
        }
        mod_l = jnp.split((jax.nn.silu(c) @ w_ada[l] + b_ada[l])[:, None, :], 6, axis=-1)
        mod_c = jnp.split((jax.nn.silu(c_ctx) @ w_ada[l] + b_ada[l])[None, None, :], 6, axis=-1)
        h_lat = modulate(rmsnorm(x, norm1_g[l]), mod_l[0], mod_l[1])
        h_ctx = modulate(rmsnorm(xc, norm1_g[l]), mod_c[0], mod_c[1])
        if last:
            hf_c, hb_c = lru_scans(h_ctx @ p['w_in'][:, OFF_LRU_REC:OFF_SC], p, zeros_state, zeros_state)
            state_f, state_b = hf_c[:, -1], hb_c[:, 0]
        else:
            out_c, state_f, state_b = mixer_sublayer(h_ctx, p, zeros_state, zeros_state)
            xc = xc + mod_c[2] * out_c
            hc2 = modulate(rmsnorm(xc, norm2_g[l]), mod_c[3], mod_c[4])
            xc = xc + mod_c[5] * ec_moe(hc2, w_router[l], w_exp_gate[l], w_exp_up[l], w_exp_down[l])
        out_l, _, _ = mixer_sublayer(h_lat, p, state_f, state_b)
        x = x + mod_l[2] * out_l
        h2 = modulate(rmsnorm(x, norm2_g[l]), mod_l[3], mod_l[4])
        x = x + mod_l[5] * ec_moe(h2, w_router[l], w_exp_gate[l], w_exp_up[l], w_exp_down[l])
    return rmsnorm(x, final_norm_g)
```

```python
import math
import numpy as np
import ml_dtypes
import concourse.bass as bass
import concourse.mybir as mybir
from concourse.bass_utils import run_bass_kernel_spmd

F32 = mybir.dt.float32
BF16 = mybir.dt.bfloat16
I32 = mybir.dt.int32
AF = mybir.ActivationFunctionType
ALU = mybir.AluOpType

D = 1024
SEQ = 2048
CTXL = 256
DEPTH = 2
NE = 16
DEXP = 2816
NFC = DEXP // 128
OFF_LRU_GATE = 3 * D
OFF_LRU_REC = OFF_LRU_GATE + D
OFF_SC = OFF_LRU_REC + D
OFF_GATE = OFF_SC + 3 * D
DIN = OFF_GATE + 3 * D
EPS = 1e-6
CG = 256
PE_ALT = "dve"


CUR_PROG = [None]


class Buf:
    __slots__ = ("name", "w", "r", "sem", "semval", "sg")

    def __init__(self, name="", sg=None):
        self.name = name
        self.sg = sg
        self.w = None
        self.r = dict(CUR_PROG[0].fence) if CUR_PROG[0] is not None else {}
        self.sem = None
        self.semval = 0


class T:
    __slots__ = ("ap", "bufs")

    def __init__(self, ap, bufs):
        self.ap = ap
        self.bufs = bufs if isinstance(bufs, (list, tuple)) else [bufs]

    def __getitem__(self, idx):
        return T(self.ap[idx], self.bufs)

    def v(self, ap):
        return T(ap, self.bufs)


def _ap(x):
    return x.ap if isinstance(x, T) else x


def _bufs(xs):
    out = []
    for x in xs:
        if isinstance(x, T):
            out.extend(x.bufs)
    return out


class Prog:
    def __init__(self):
        self.nc = bass.Bass("TRN2", target_bir_lowering=False)
        nc = self.nc
        self.fence = {}
        CUR_PROG[0] = None
        self.eng = {"pe": nc.tensor, "act": nc.scalar, "dve": nc.vector, "pool": nc.gpsimd, "sp": nc.sync}
        self.sem = {e: nc.alloc_semaphore("s_" + e) for e in ("pe", "act", "dve", "pool")}
        self.cnt = {e: 0 for e in self.sem}
        self.known = {e: {} for e in self.eng}
        self.vc = {}
        self.semreg = {}
        self.nbuf = 0
        self.AW = 52992
        self.arena = nc.alloc_sbuf_tensor("arena", [128, self.AW], F32).ap()
        self.top = 0
        self.hw = 0
        self.peaks = {}
        self.banks = []
        for i in range(8):
            ap = nc.alloc_psum_tensor("bank%d" % i, [128, 512], F32).ap()
            self.banks.append(T(ap, Buf("bank%d" % i)))
        self.bank_i = 0
        self.reserved = set()
        CUR_PROG[0] = self

    def alloc(self, words, name=""):
        words = (words + 7) // 8 * 8
        off = self.top
        self.top += words
        self.hw = max(self.hw, self.top)
        assert self.top <= self.AW, "SBUF arena overflow %s: %d > %d" % (name, self.top, self.AW)
        return off

    def tile(self, shape, dt=F32, name="", nb=None, sg=None):
        free = int(np.prod(shape[1:]))
        words = free if dt in (F32, I32) else (free + 1) // 2
        off = self.alloc(words, name)
        ap = self.arena[0:shape[0], off:off + words]
        if dt != F32:
            ap = ap.bitcast(dt)
            if ap.shape[1] != free:
                ap = ap[:, 0:free]
        if len(shape) == 3:
            ap = ap.rearrange("p (a b) -> p a b", b=shape[2])
        elif len(shape) == 4:
            ap = ap.rearrange("p (a b c) -> p a b c", b=shape[2], c=shape[3])
        return T(ap, nb if nb is not None else Buf(name, sg))

    def mark(self):
        return self.top

    def peak(self, name):
        self.peaks[name] = max(self.peaks.get(name, 0), self.hw)
        self.hw = self.top

    def release(self, m):
        self.top = m
        f = {}
        for e in self.cnt:
            if self.cnt[e] > 0:
                f[e] = (e, self.cnt[e])
        for nm, ent in self.semreg.items():
            if ent[1] > 0:
                f[nm] = ("dma", nm, ent[1], ent[0])
        self.fence = f

    def bank(self):
        while True:
            b = self.banks[self.bank_i]
            self.bank_i = (self.bank_i + 1) % 8
            if id(b) not in self.reserved:
                return b

    def bank_reserve(self):
        b = self.bank()
        self.reserved.add(id(b))
        return b

    def bank_free(self, b):
        self.reserved.discard(id(b))

    def _key(self, tok):
        return tok[0] if tok[0] != "dma" else tok[1]

    def _wait(self, E, toks):
        need = {}
        for tok in toks:
            if tok is None:
                continue
            k = self._key(tok)
            val = self.semreg[k][1] if tok[0] == "dma" else tok[1]
            if k not in need or need[k][0] < val:
                need[k] = (val, tok)
        kn = self.known[E]
        for k, (val, tok) in need.items():
            if kn.get(k, 0) >= val:
                continue
            semh = tok[3] if tok[0] == "dma" else self.sem[tok[0]]
            self.eng[E].wait_ge(semh, val)
            kn[k] = val
            snap = self.vc.get(tok[:3])
            if snap:
                for kk, vv in snap.items():
                    if kn.get(kk, 0) < vv:
                        kn[kk] = vv

    def _deps(self, reads, writes, E=None, dj=False):
        toks = []
        for b in reads:
            if b.w is not None:
                toks.append(b.w)
        for b in writes:
            if b.w is not None and not (dj and b.w[0] == E):
                toks.append(b.w)
            toks.extend(b.r.values())
        return toks

    def _commit(self, tok, E, reads, writes):
        self.vc[tok[:3]] = dict(self.known[E])
        k = self._key(tok)
        for b in reads:
            b.r[k] = tok
        for b in writes:
            b.w = tok
            b.r = {}

    def op(self, E, fn, reads, writes, dj=False):
        reads = _bufs(reads)
        writes = _bufs(writes)
        self._wait(E, self._deps(reads, writes, E, dj))
        ins = fn(self.eng[E])
        self.cnt[E] += 1
        ins.then_inc(self.sem[E], 1)
        tok = (E, self.cnt[E])
        self._commit(tok, E, reads, writes)
        return tok

    def dma(self, q, out, in_, sembuf=None, disjoint=False):
        reads = _bufs([in_])
        writes = _bufs([out])
        deps = self._deps(reads, writes)
        if disjoint:
            skip = set(id(b.w) for b in writes if b.w is not None and b.w[0] == "dma")
            deps = [t for t in deps if id(t) not in skip]
        self._wait(q, deps)
        sb = sembuf if sembuf is not None else writes[0]
        key = sb.sg or sb.name
        assert key, "dma target Buf needs a name"
        ent = self.semreg.get(key)
        if ent is None:
            ent = [self.nc.alloc_semaphore("d%d" % len(self.semreg)), 0]
            self.semreg[key] = ent
        ent[1] += 16
        self.eng[q].dma_start(out=_ap(out), in_=_ap(in_)).then_inc(ent[0], 16)
        tok = ("dma", key, ent[1], ent[0])
        self._commit(tok, q, reads, writes)
        return tok

    def mm(self, out, terms):
        reads = []
        for l, r in terms:
            reads += [l, r]
        n = len(terms)

        def fn(pe):
            ins = None
            for i, (l, r) in enumerate(terms):
                ins = pe.matmul(_ap(out), _ap(l), _ap(r), start=(i == 0), stop=(i == n - 1))
            return ins
        return self.op("pe", fn, reads, [out])

    def mm_acc(self, out, terms, start, stop):
        reads = []
        for l, r in terms:
            reads += [l, r]
        n = len(terms)

        def fn(pe):
            ins = None
            for i, (l, r) in enumerate(terms):
                ins = pe.matmul(_ap(out), _ap(l), _ap(r), start=(start and i == 0), stop=(stop and i == n - 1))
            return ins
        return self.op("pe", fn, reads, [out])

    def transpose(self, out, in_, ident):
        return self.op("pe", lambda pe: pe.transpose(_ap(out), _ap(in_), _ap(ident)), [in_, ident], [out])

    def act(self, out, in_, func, bias=None, scale=1.0, accum=None, dj=False):
        reads = [in_] + ([bias] if isinstance(bias, T) else []) + ([scale] if isinstance(scale, T) else [])
        writes = [out] + ([accum] if accum is not None else [])
        kw = {}
        if bias is not None:
            kw["bias"] = _ap(bias)
        if accum is not None:
            kw["accum_out"] = _ap(accum)
        return self.op("act", lambda e: e.activation(_ap(out), _ap(in_), func, scale=_ap(scale), **kw), reads, writes, dj=dj)

    def tt(self, out, a, b, op, E="dve", dj=False):
        return self.op(E, lambda e: e.tensor_tensor(_ap(out), _ap(a), _ap(b), op), [a, b], [out], dj=dj)

    def ts(self, out, a, s1, s2, op0, op1=None, E="dve", accum=None, dj=False):
        reads = [a] + [s for s in (s1, s2) if isinstance(s, T)]
        writes = [out] + ([accum] if accum is not None else [])
        if op1 is None:
            return self.op(E, lambda e: e.tensor_scalar(_ap(out), _ap(a), _ap(s1), None, op0), reads, writes, dj=dj)
        kw = {"accum_out": _ap(accum)} if accum is not None else {}
        return self.op(E, lambda e: e.tensor_scalar(_ap(out), _ap(a), _ap(s1), _ap(s2), op0, op1, **kw), reads, writes, dj=dj)

    def stt(self, out, a, s, b, op0, op1):
        reads = [a, b] + ([s] if isinstance(s, T) else [])
        return self.op("dve", lambda e: e.scalar_tensor_tensor(_ap(out), _ap(a), _ap(s), _ap(b), op0, op1), reads, [out])

    def copy(self, out, in_, E="dve"):
        if E == "act":
            return self.act(out, in_, AF.Copy)
        return self.op(E, lambda e: e.tensor_copy(_ap(out), _ap(in_)), [in_], [out])

    def memset(self, out, val, E="dve"):
        return self.op(E, lambda e: e.memset(_ap(out), val), [], [out])

    def scan(self, out, a, b, init):
        reads = [a, b] + ([init] if isinstance(init, T) else [])
        return self.op("dve", lambda e: e.tensor_tensor_scan(_ap(out), _ap(a), _ap(b), _ap(init), ALU.mult, ALU.add), reads, [out])

    def recip(self, out, in_):
        return self.op("dve", lambda e: e.reciprocal(_ap(out), _ap(in_)), [in_], [out])


def col(v):
    v = np.asarray(v, np.float32).reshape(-1, 128)
    return np.ascontiguousarray(v.T)


def bf(a):
    return np.ascontiguousarray(np.asarray(a, np.float32).astype(ml_dtypes.bfloat16))


def sincos_1d(pos, dim):
    half = dim // 2
    omega = 1.0 / (10000.0 ** (np.arange(half, dtype=np.float32) / np.float32(half)))
    ang = pos[:, None].astype(np.float32) * omega[None, :].astype(np.float32)
    return np.concatenate([np.sin(ang), np.cos(ang)], axis=-1).astype(np.float32)


def grid_pos_embed(rows, gw=64):
    half = D // 2
    er = sincos_1d(np.arange(rows, dtype=np.float32), half)
    ec = sincos_1d(np.arange(gw, dtype=np.float32), half)
    emb = np.concatenate([np.broadcast_to(er[:, None, :], (rows, gw, half)),
                          np.broadcast_to(ec[None, :, :], (rows, gw, half))], axis=-1)
    return emb.reshape(rows * gw, D).astype(np.float32)


def dft_consts(L):
    NTc = L // 128
    f = np.arange(L, dtype=np.int64)
    t = np.arange(L, dtype=np.int64)
    ph = ((2 * f[:, None] + 1) * t[None, :]) % (4 * L)
    ang = ph.astype(np.float64) * (2.0 * np.pi / (4 * L))
    C = np.cos(ang)
    S = np.sin(ang)
    def fwd(M):
        return M.reshape(NTc, 128, NTc, 128).transpose(0, 3, 2, 1)
    FWD = np.stack([fwd(C), fwd(S)], axis=2)
    INV = np.stack([C.reshape(NTc, 128, L) / L, -S.reshape(NTc, 128, L) / L], axis=2)
    return bf(FWD), bf(INV)


def filt_consts(L):
    bands = 16
    t01 = np.linspace(0.0, 1.0, L, dtype=np.float32)[:, None]
    w = (np.float32(2.0 * math.pi / L) * np.arange(L, dtype=np.float32))[:, None]
    fr = np.linspace(1e-4, bands - 1, bands, dtype=np.float32)[None, :]
    feats = np.concatenate([t01, np.cos(fr * w), -np.sin(fr * w)], axis=-1).astype(np.float32)
    mn = math.log(1e-2) / 1.5
    mx = math.log(1e-2) / 0.3
    deltas = np.linspace(mn, mx, D, dtype=np.float32)
    decay = np.exp(-t01 * np.abs(deltas)[None, :]).astype(np.float32)
    featsT = np.ascontiguousarray(feats.T)
    decay_l = np.ascontiguousarray(decay.reshape(L // 128, 128, D).transpose(1, 0, 2))
    return featsT, decay_l


class Stream:
    def __init__(self, name, L):
        self.name = name
        self.L = L
        self.NT = L // 128
        self.SL = min(512, L)
        self.NS = L // self.SL


class Builder:
    def __init__(self, dbg=(), stop=None):
        self.P = Prog()
        self.nc = self.P.nc
        self.inputs = {}
        self.dbg_req = set(dbg)
        self.dbg_out = {}
        self.stop = stop

    def inp(self, name, shape, dt=F32):
        if name not in self.inputs:
            ap = self.nc.dram_tensor(name, list(shape), dt, kind="ExternalInput").ap()
            self.inputs[name] = T(ap, Buf(name))
        return self.inputs[name]

    def scratch(self, name, shape, dt=F32):
        ap = self.nc.dram_tensor(name, list(shape), dt, kind="Internal").ap()
        return T(ap, Buf(name))

    def dbg(self, name, t, shape=None):
        if name not in self.dbg_req:
            return
        P = self.P
        shp = list(t.ap.shape)
        o = self.nc.dram_tensor("dbg_" + name, shp, t.ap.dtype, kind="ExternalOutput").ap()
        ob = T(o, Buf("dbg_" + name, "dbg"))
        P.dma("sp", ob, t)
        self.dbg_out[name] = ob

    def finish(self, outs):
        P = self.P
        toks = []
        for o in list(outs) + list(self.dbg_out.values()):
            for b in o.bufs:
                if b.w is not None:
                    toks.append(b.w)
        P._wait("sp", toks)


def build_consts(B):
    P = B.P
    c = {}
    c["ident_bf"] = P.tile([128, 128], BF16, "ident_bf", sg="cols")
    P.dma("sp", c["ident_bf"], B.inp("c_ident_bf", [128, 128], BF16))
    c["ident_f"] = P.tile([128, 128], F32, "ident_f", sg="cols")
    P.dma("sp", c["ident_f"], B.inp("c_ident_f", [128, 128], F32))
    c["eps"] = P.tile([128, 1], F32, "eps")
    P.memset(c["eps"], EPS)
    c["one"] = P.tile([128, 1], F32, "one")
    P.memset(c["one"], 1.0)
    c["ones_bf"] = P.tile([128, 128], BF16, "ones_bf")
    P.memset(c["ones_bf"], 1.0)
    c["ones_f"] = P.tile([128, 128], F32, "ones_f")
    P.memset(c["ones_f"], 1.0)
    return c


def load_cols(B, name, n, dt=F32):
    t = B.P.tile([128, n], dt, name, sg="cols")
    B.P.dma("sp", t, B.inp(name, [128, n], dt))
    return t


def adaln(B, C):
    P = B.P
    cc = load_cols(B, "cvec", 16)
    sil = P.tile([128, 8, 2], BF16, "silu_c")
    silf = P.tile([128, 16], F32, "silu_cf")
    P.act(silf, cc, AF.Silu)
    P.copy(sil.v(sil.ap[:, :, 0]), silf[:, 0:8])
    P.copy(sil.v(sil.ap[:, :, 1]), silf[:, 8:16])
    outs = [P.tile([128, 48, 2], F32, "mod%d" % l) for l in range(DEPTH)]
    bcols = [load_cols(B, "b_ada_c%d" % l, 48) for l in range(DEPTH)]
    m1 = P.mark()
    ring = [P.tile([128, 8, 512], BF16, "wada%d" % i) for i in range(3)]
    wsrc = B.inp("w_ada", [DEPTH, D, 6 * D], F32)
    ri = 0
    import os
    NL_ = int(os.environ.get("ADA_NL", DEPTH)); NJ_ = int(os.environ.get("ADA_NJ", 12))
    for l in range(NL_):
        for js in range(NJ_):
            w = ring[ri % 3]
            ri += 1
            src = wsrc.v(wsrc.ap[l, :, js * 512:(js + 1) * 512].rearrange("(k p) j -> p k j", p=128))
            P.dma("pool", w, src)
            for jj in range(4):
                j = js * 4 + jj
                ps = P.bank()
                pv = ps[:, 0:2]
                P.mm(pv, [(w[:, k, jj * 128:(jj + 1) * 128], sil[:, k, :]) for k in range(8)])
                P.act(outs[l][:, j, :], pv, AF.Identity, bias=bcols[l][:, j:j + 1], dj=(j > 0))
    P.release(m1)
    return outs


def norm_to_hT(B, C, st, Xd, Xdb, gcol, shiftcol, hT, hT_bufs):
    P = B.P
    m = P.mark()
    junk = P.tile([128, 1024], F32, "nrm_junk")
    xt = [P.tile([128, 1024], F32, "nrm_x%d" % i) for i in range(2)]
    xn = [P.tile([128, 1024], BF16, "nrm_xn%d" % i) for i in range(2)]
    ss = [P.tile([128, 1], F32, "nrm_ss%d" % i) for i in range(2)]
    sq = [P.tile([128, 1], F32, "nrm_sq%d" % i) for i in range(2)]
    rs = [P.tile([128, 1], F32, "nrm_rs%d" % i) for i in range(2)]
    idn = C["ident_bf"]
    for c in range(st.NT):
        i = c % 2
        P.dma("sp", xt[i], T(Xd.ap[c * 128:(c + 1) * 128, :], Xdb[c]))
        P.act(junk, xt[i], AF.Square, accum=ss[i])
        P.act(sq[i], ss[i], AF.Sqrt, bias=C["eps"], scale=1.0 / D)
        P.recip(rs[i], sq[i])
        P.ts(xn[i], xt[i], rs[i], None, ALU.mult)
        ps = P.bank()
        pb = ps.v(ps.ap.bitcast(BF16))
        xni = xn[i]

        def tr(pe, pb=pb, xni=xni):
            ins = None
            for k in range(8):
                ins = pe.transpose(pb.ap[:, k * 128:(k + 1) * 128], xni.ap[:, k * 128:(k + 1) * 128], idn.ap)
            return ins
        P.op("pe", tr, [xni, idn], [pb])
        sl = (c * 128) // st.SL
        for k in range(8):
            dst = T(hT.ap[:, k, c * 128:(c + 1) * 128], hT_bufs[k][c])
            if c % 2 == 0:
                P.act(dst, pb[:, k * 128:(k + 1) * 128], AF.Identity, bias=shiftcol[:, k:k + 1], scale=gcol[:, k:k + 1])
            else:
                P.ts(dst, pb[:, k * 128:(k + 1) * 128], gcol[:, k:k + 1], shiftcol[:, k:k + 1], ALU.mult, ALU.add)
    P.release(m)


def hT_alloc(P, st, name):
    hT = P.tile([128, 8, st.L], BF16, name)
    bufs = [[Buf("%s_%d_%d" % (name, k, c)) for c in range(st.NT)] for k in range(8)]
    return hT, bufs


def hT_slab(hT, bufs, k, s, st, lo=None, hi=None):
    cps = st.SL // 128
    return T(hT.ap[:, k, s * st.SL:(s + 1) * st.SL], bufs[k][s * cps:(s + 1) * cps])


class Ring:
    def __init__(self, P, n, shape, dt, name):
        self.tiles = [P.tile(shape, dt, "%s%d" % (name, i)) for i in range(n)]
        self.i = 0

    def next(self):
        t = self.tiles[self.i % len(self.tiles)]
        self.i += 1
        return t


class MixCtx:
    pass


def load_win(M, col0, ncol, ring):
    w = ring.next()
    src = M.win.v(M.win.ap[M.l, :, col0:col0 + ncol].rearrange("(k p) j -> p k j", p=128))
    M.P.dma("pool", w[:, :, 0:ncol], src)
    return w


def make_diag(M, out_bf, wcol):
    M.P.ts(out_bf, M.C["ident_f"], wcol, None, ALU.mult)


def hslab(M, k, s):
    st = M.st
    cps = st.SL // 128
    return T(M.hT.ap[:, k, s * st.SL:(s + 1) * st.SL], M.hTb[k][s * cps:(s + 1) * cps])


def yslab(M, k, s):
    st = M.st
    return T(M.yT.ap[:, k, s * st.SL:(s + 1) * st.SL], M.yTb[k])


def proj_conv(M, w, wofs, ntap, left, wcols, bcol, ppad, out, evac_out):
    P, st = M.P, M.st
    L, SL, NS = st.L, st.SL, st.NS
    if left > 0:
        P.memset(ppad[:, 0:left], 0.0)
    if ntap - 1 - left > 0:
        P.memset(ppad[:, left + L:L + ntap - 1], 0.0)
    for s in range(NS):
        ps = P.bank()
        P.mm(ps[:, 0:SL], [(w[:, k, wofs:wofs + 128], hslab(M, k, s)) for k in range(8)])
        P.act(ppad[:, left + s * SL:left + (s + 1) * SL], ps[:, 0:SL], AF.Copy, dj=(s > 0))
    dg = [M.dgring.next() for _ in range(ntap)]
    for tp in range(ntap):
        make_diag(M, dg[tp], wcols[tp])
    for s in range(NS):
        ps = P.bank()
        P.mm(ps[:, 0:SL], [(dg[tp], ppad[:, s * SL + tp:s * SL + tp + SL]) for tp in range(ntap)])
        evac_out(out[:, s * SL:(s + 1) * SL], ps[:, 0:SL], s)


def hyena_path(M):
    P, st, C, B, l = M.P, M.st, M.C, M.B, M.l
    L, NT, SL, NS = st.L, st.NT, st.SL, st.NS
    NF = NT
    m0 = P.mark()
    cw = load_cols(B, "hy_conv_w_c%d" % l, 72)
    cb = load_cols(B, "hy_conv_b_c%d" % l, 24)
    wring = Ring(P, 2, [128, 8, CG], BF16, "hywin")
    ppad = [P.tile([128, L + 2], BF16, "hyppad%d" % i) for i in range(2)]
    uT = P.tile([128, 2, L], BF16, "hy_uT")
    uTb = [Buf("hy_uT%d" % i) for i in range(2)]
    mT = P.tile([128, 2, L], BF16, "hy_mT")
    mTb = [Buf("hy_mT%d" % i) for i in range(2)]
    vtok = P.tile([128, NT, CG], BF16, "hy_vtok")
    Y = P.tile([128, NF, 2, CG], BF16, "hy_Y")
    NFR = 3
    fring = Ring(P, NFR, [128, 2 * L], BF16, "hy_dft")
    kring = Ring(P, NFR, [128, 2, 2 * CG], F32, "hy_k")
    tq = [P.tile([128, CG], F32, "hy_t%d" % i) for i in range(4)]
    fwd_src = B.inp("c_fwd%d" % L, [NT, 128, 2, NT, 128], BF16)
    inv_src = B.inp("c_inv%d" % L, [NT, 128, 2, L], BF16)
    Kscr, Kb = M.Ks
    evi = [0]

    def conv_into(dstT, dstb, base, g):
        w = load_win(M, base + g * CG, CG, wring)
        for m in range(2):
            cj = base // 128 + g * 2 + m
            wcols = [cw[:, tp * 24 + cj:tp * 24 + cj + 1] for tp in range(3)]
            bcol = cb[:, cj:cj + 1]
            out = T(dstT.ap[:, m, :], dstb[m])
            proj_conv(M, w, m * 128, 3, 1, wcols, bcol, ppad[m], out,
                      lambda d, p, s, bcol=bcol: P.act(d, p, AF.Identity, bias=bcol, dj=(s > 0)))

    def to_tok(srcT, srcb):
        gs = min(4, NT)
        for cq in range(NT // gs):
            ps = P.bank()
            pb = ps.v(ps.ap.bitcast(BF16))
            idn = C["ident_bf"]
            srcs = [T(srcT.ap[:, m, :], srcb[m]) for m in range(2)]

            def tr(pe, cq=cq, pb=pb):
                ins = None
                for cc in range(gs):
                    for m in range(2):
                        t0 = (cq * gs + cc) * 128
                        ins = pe.transpose(pb.ap[:, cc * CG + m * 128:cc * CG + (m + 1) * 128],
                                           srcT.ap[:, m, t0:t0 + 128], idn.ap)
                return ins
            P.op("pe", tr, srcs + [idn], [pb])
            dst = vtok.v(vtok.ap[:, cq * gs:(cq + 1) * gs, :].rearrange("p a b -> p (a b)"))
            evi[0] += 1
            P.act(dst, pb[:, 0:gs * CG], AF.Copy, dj=(cq > 0))

    def fft_conv(g, o, mulT, mulb, dst_fn):
        fws, kts, ivs = {}, {}, {}

        def pref_f(i):
            if 0 <= i < NF:
                fw = fring.next()
                P.dma("sp", fw, fwd_src.v(fwd_src.ap[i].rearrange("p a c j -> p (a c j)")))
                fws[i] = fw
                kt = kring.next()
                P.dma("sp", kt, T(Kscr.ap[g, i], Kb[g]))
                kts[i] = kt

        def pref_i(i):
            if 0 <= i < NF and i not in ivs:
                iv = fring.next()
                P.dma("sp", iv, inv_src.v(inv_src.ap[i].rearrange("p a t -> p (a t)")))
                ivs[i] = iv
        for i in range(NFR - 1):
            pref_f(i)
        for i in range(NF):
            pref_f(i + NFR - 1)
            pref_i(i + NFR - 1 - NF)
            fw = fws.pop(i)
            fwv = fw.v(fw.ap.rearrange("p (a c j) -> p a c j", a=2, c=NT))
            kt = kts.pop(i)
            pc = P.bank()
            pss = P.bank()
            P.mm(pc[:, 0:CG], [(fwv[:, 0, c, :], vtok[:, c, :]) for c in range(NT)])
            P.mm(pss[:, 0:CG], [(fwv[:, 1, c, :], vtok[:, c, :]) for c in range(NT)])
            kre = kt[:, 0, o * CG:(o + 1) * CG]
            kim = kt[:, 1, o * CG:(o + 1) * CG]
            P.tt(tq[0], pc[:, 0:CG], kre, ALU.mult)
            P.tt(tq[1], pss[:, 0:CG], kim, ALU.mult)
            P.tt(Y[:, i, 0, :], tq[0], tq[1], ALU.add, dj=True)
            P.tt(tq[2], pc[:, 0:CG], kim, ALU.mult)
            P.tt(tq[3], pss[:, 0:CG], kre, ALU.mult)
            P.tt(Y[:, i, 1, :], tq[2], tq[3], ALU.subtract, dj=True)
        acc = [[P.bank_reserve() for n in range(NS)] for m in range(2)]
        for i in range(NF):
            for j in range(i, i + NFR):
                pref_i(j)
            iv = ivs.pop(i)
            ivv = iv.v(iv.ap.rearrange("p (a t) -> p a t", a=2))
            for m in range(2):
                for n in range(NS):
                    P.mm_acc(acc[m][n][:, 0:SL],
                             [(Y[:, i, 0, m * 128:(m + 1) * 128], ivv[:, 0, n * SL:(n + 1) * SL]),
                              (Y[:, i, 1, m * 128:(m + 1) * 128], ivv[:, 1, n * SL:(n + 1) * SL])],
                             start=(i == 0), stop=(i == NF - 1))
        for m in range(2):
            for n in range(NS):
                P.tt(dst_fn(m, n), acc[m][n][:, 0:SL], T(mulT.ap[:, m, n * SL:(n + 1) * SL], mulb[m]), ALU.mult, dj=(n > 0))
                P.bank_free(acc[m][n])

    for g in range(D // CG):
        conv_into(uT, uTb, 2 * D, g)
        to_tok(uT, uTb)
        conv_into(mT, mTb, 0, g)
        if g == 0:
            B.dbg("hy_vT_%s%d" % (st.name, l), T(uT.ap, uTb))
        fft_conv(g, 0, mT, mTb, lambda m, n: T(uT.ap[:, m, n * SL:(n + 1) * SL], uTb[m]))
        to_tok(uT, uTb)
        conv_into(mT, mTb, D, g)
        fft_conv(g, 1, mT, mTb, lambda m, n, g=g: T(M.yT.ap[:, g * 2 + m, n * SL:(n + 1) * SL], M.yTb[g * 2 + m]))
    P.release(m0)


def lru_path(M, only_states=False):
    P, st, C, B, l = M.P, M.st, M.C, M.B, M.l
    L, NT, SL, NS = st.L, st.NT, st.SL, st.NS
    m0 = P.mark()
    cw = load_cols(B, "lru_conv_w_c%d" % l, 32)
    cb = load_cols(B, "lru_conv_b_c%d" % l, 8)
    ba = load_cols(B, "lru_ba_c%d" % l, 16)
    bx = load_cols(B, "lru_bx_c%d" % l, 16)
    lam = load_cols(B, "lru_lambda_c%d" % l, 16)
    nsp = P.tile([128, 16], F32, "lru_nsp")
    P.act(nsp, lam, AF.Exp, scale=-1.0)
    P.act(nsp, nsp, AF.Ln, bias=C["one"])
    P.ts(nsp, nsp, -8.0, None, ALU.mult)
    wring = Ring(P, 2, [128, 8, 128], BF16, "lruwin")
    gring = Ring(P, 1, [128, 8, 128], BF16, "lruwing")
    bdring = Ring(P, 8, [128, 128], BF16, "lrubd")
    for t_ in bdring.tiles:
        P.memset(t_, 0.0)
    wa_src = B.inp("lru_wa", [DEPTH, 2, 16, 64, 64], F32)
    wx_src = B.inp("lru_wx", [DEPTH, 2, 16, 64, 64], F32)
    ppad = P.tile([128, L + 3], BF16, "lruppad")
    xcf = P.tile([128, L], F32, "lru_xcf")
    xcb = P.tile([128, L], BF16, "lru_xcb")
    rr = [P.tile([128, L], F32, "lru_r%d" % d_) for d_ in range(2)]
    ii = [P.tile([128, L], F32, "lru_i%d" % d_) for d_ in range(2)]
    aa = [P.tile([128, L], F32, "lru_a%d" % d_) for d_ in range(2)]
    bb = [P.tile([128, L], F32, "lru_b%d" % d_) for d_ in range(2)]
    a_, b_ = aa[0], bb[0]
    hh = [P.tile([128, L], F32, "lru_h%d" % d_) for d_ in range(2)]
    for k in range(8):
        w = load_win(M, OFF_LRU_REC + k * 128, 128, wring)
        wcols = [cw[:, tp * 8 + k:tp * 8 + k + 1] for tp in range(4)]
        bcol = cb[:, k:k + 1]
        proj_conv(M, w, 0, 4, 2, wcols, bcol, ppad, xcf,
                  lambda d, p, s, bcol=bcol: P.act(d, p, AF.Identity, bias=bcol, dj=(s > 0)))
        P.copy(xcb, xcf, E=PE_ALT)
        import os
        CUT = int(os.environ.get("LRU_CUT", 99))
        if CUT == 1:
            break
        for dr in range(2):
            r_, i_, a_, b_ = rr[dr], ii[dr], aa[dr], bb[dr]
            bds = []
            for (src, nm) in ((wa_src, "a"), (wx_src, "x")):
                bd = bdring.next()
                for h in range(2):
                    P.dma("pool", bd[h * 64:(h + 1) * 64, h * 64:(h + 1) * 64], src.v(src.ap[l, dr, 2 * k + h]), disjoint=(h == 1))
                bds.append(bd)
            for s in range(NS):
                pa = P.bank()
                P.mm(pa[:, 0:SL], [(bds[0], xcb[:, s * SL:(s + 1) * SL])])
                P.act(r_[:, s * SL:(s + 1) * SL], pa[:, 0:SL], AF.Sigmoid, bias=ba[:, dr * 8 + k:dr * 8 + k + 1], dj=(s > 0))
                px = P.bank()
                P.mm(px[:, 0:SL], [(bds[1], xcb[:, s * SL:(s + 1) * SL])])
                P.act(i_[:, s * SL:(s + 1) * SL], px[:, 0:SL], AF.Sigmoid, bias=bx[:, dr * 8 + k:dr * 8 + k + 1], dj=(s > 0))
            if CUT == 2:
                break
            P.act(a_, r_, AF.Exp, scale=nsp[:, dr * 8 + k:dr * 8 + k + 1])
            P.act(r_, a_, AF.Square)
            P.act(r_, r_, AF.Sqrt, bias=C["one"], scale=-1.0)
            P.tt(b_, r_, i_, ALU.mult, E=PE_ALT)
            P.tt(b_, b_, xcf, ALU.mult, E=PE_ALT)
            if CUT == 3:
                break
            h_ = hh[dr]
            if st.name == "ctx":
                init = 0.0
            else:
                init = (M.stF if dr == 0 else M.stB)[:, k:k + 1]
            if dr == 0:
                P.scan(h_, a_, b_, init)
                if st.name == "ctx":
                    P.copy(M.stF[:, k:k + 1], h_[:, L - 1:L])
            else:
                P.scan(h_.v(h_.ap[:, ::-1]), a_.v(a_.ap[:, ::-1]), b_.v(b_.ap[:, ::-1]), init)
                if st.name == "ctx":
                    P.copy(M.stB[:, k:k + 1], h_[:, 0:1])
        if CUT <= 4:
            break
        if k == 0:
            B.dbg("lru_hf_%s%d" % (st.name, l), hh[0])
            B.dbg("lru_hb_%s%d" % (st.name, l), hh[1])
        if only_states:
            continue
        P.tt(hh[0], hh[0], hh[1], ALU.add, E=PE_ALT)
        wg = load_win(M, OFF_LRU_GATE + k * 128, 128, gring)
        a_, b_ = aa[0], bb[0]
        gx = a_
        for s in range(NS):
            pg = P.bank()
            P.mm(pg[:, 0:SL], [(wg[:, kk, 0:128], hslab(M, kk, s)) for kk in range(8)])
            P.act(gx[:, s * SL:(s + 1) * SL], pg[:, 0:SL], AF.Copy, dj=(s > 0))
        if CUT == 6:
            break
        P.act(b_, gx, AF.Square)
        P.ts(b_, b_, 0.044715, 1.0, ALU.mult, ALU.add, E=PE_ALT)
        P.tt(b_, b_, gx, ALU.mult, E=PE_ALT)
        P.act(b_, b_, AF.Sigmoid, scale=1.5957691216057308)
        P.tt(b_, b_, gx, ALU.mult, E=PE_ALT)
        P.tt(T(M.yT.ap[:, k, :], M.yTb[k]), b_, hh[0], ALU.mult)
        if CUT == 5 or (CUT >= 10 and k == CUT - 10):
            break
    P.release(m0)


def sconv_path(M):
    P, st, C, B, l = M.P, M.st, M.C, M.B, M.l
    L, NT, SL, NS = st.L, st.NT, st.SL, st.NS
    m0 = P.mark()
    cw = load_cols(B, "sc_conv_w_c%d" % l, 24)
    wring = Ring(P, 4, [128, 8, 128], BF16, "scwin")
    bgs = [P.tile([128, L], BF16, "sc_bg%d" % i) for i in range(2)]
    cgs = [P.tile([128, L], F32, "sc_cg%d" % i) for i in range(2)]
    ppads = [P.tile([128, L + 2], BF16, "sc_ppad%d" % i) for i in range(2)]
    for ppad in ppads:
        P.memset(ppad[:, 0:1], 0.0)
        P.memset(ppad[:, L + 1:L + 2], 0.0)
    for k in range(8):
        bg, cg, ppad = bgs[k % 2], cgs[k % 2], ppads[k % 2]
        wb = load_win(M, OFF_SC + k * 128, 128, wring)
        wc = load_win(M, OFF_SC + D + k * 128, 128, wring)
        wx = load_win(M, OFF_SC + 2 * D + k * 128, 128, wring)
        for s in range(NS):
            sl = slice(s * SL, (s + 1) * SL)
            p1 = P.bank()
            P.mm(p1[:, 0:SL], [(wb[:, kk, 0:128], hslab(M, kk, s)) for kk in range(8)])
            P.act(bg[:, sl], p1[:, 0:SL], AF.Copy, dj=(s > 0))
            p2 = P.bank()
            P.mm(p2[:, 0:SL], [(wc[:, kk, 0:128], hslab(M, kk, s)) for kk in range(8)])
            P.act(cg[:, sl], p2[:, 0:SL], AF.Copy, dj=(s > 0))
            p3 = P.bank()
            P.mm(p3[:, 0:SL], [(wx[:, kk, 0:128], hslab(M, kk, s)) for kk in range(8)])
            P.tt(ppad[:, 1 + s * SL:1 + (s + 1) * SL], p3[:, 0:SL], cg[:, sl], ALU.mult, dj=(s > 0))
        dg = [M.dgring.next() for _ in range(3)]
        for tp in range(3):
            make_diag(M, dg[tp], cw[:, tp * 8 + k:tp * 8 + k + 1])
        for s in range(NS):
            pq = P.bank()
            P.mm(pq[:, 0:SL], [(dg[tp], ppad[:, s * SL + tp:s * SL + tp + SL]) for tp in range(3)])
            P.tt(T(M.yT.ap[:, k, s * SL:(s + 1) * SL], M.yTb[k]), pq[:, 0:SL], bg[:, s * SL:(s + 1) * SL], ALU.mult, dj=(s > 0))
    P.release(m0)


def merge_path(M, pi, wname):
    P, st, C, B, l = M.P, M.st, M.C, M.B, M.l
    L, NT, SL, NS = st.L, st.NT, st.SL, st.NS
    m0 = P.mark()
    oring = Ring(P, 2, [128, 8, 128], BF16, "mg_wo")
    gring = Ring(P, 2, [128, 8, 128], BF16, "mg_wg")
    sg = [P.tile([128, SL], F32, "mg_sg%d" % i) for i in range(2)]
    tmp = [P.tile([128, SL], BF16, "mg_tmp%d" % i) for i in range(2)]
    wsrc = B.inp(wname, [DEPTH, D, D], F32)
    it = 0
    for j in range(8):
        wo = oring.next()
        P.dma("pool", wo, wsrc.v(wsrc.ap[l, :, j * 128:(j + 1) * 128].rearrange("(k p) j -> p k j", p=128)))
        wg = load_win(M, OFF_GATE + pi * D + j * 128, 128, gring)
        for s in range(NS):
            p1 = P.bank()
            P.mm(p1[:, 0:SL], [(wo[:, k, :], yslab(M, k, s)) for k in range(8)])
            p2 = P.bank()
            P.mm(p2[:, 0:SL], [(wg[:, k, 0:128], hslab(M, k, s)) for k in range(8)])
            sgi = sg[it % 2]
            tmi = tmp[it % 2]
            it += 1
            P.act(sgi, p2[:, 0:SL], AF.Sigmoid)
            dst = T(M.merged.ap[:, j, s * SL:(s + 1) * SL], M.mergedb[j][s])
            if pi == 0:
                P.tt(dst, p1[:, 0:SL], sgi, ALU.mult)
            else:
                P.tt(tmi, p1[:, 0:SL], sgi, ALU.mult)
                P.tt(dst, dst, tmi, ALU.add, E=PE_ALT)
    P.release(m0)


def bcast_cols(M, dst, cols8):
    P, C = M.P, M.C
    m0 = P.mark()
    dg = P.tile([128, 8, 128], F32, "bc_diag")
    for k in range(8):
        P.ts(dg[:, k, :], C["ident_f"], cols8[:, k:k + 1], None, ALU.mult)
    for h in range(2):
        ps = P.bank()
        P.mm(ps, [(C["ones_f"], dg.v(dg.ap[:, h * 4:(h + 1) * 4, :].rearrange("p a b -> p (a b)")))])
        P.copy(dst[:, h * 512:(h + 1) * 512], ps, E="act")
    P.release(m0)


def out_residual(M, Xd, Xdb, modcol):
    P, st, C, B, l = M.P, M.st, M.C, M.B, M.l
    m0 = P.mark()
    bc = P.tile([128, D], F32, "or_bc")
    bcast_cols(M, bc, modcol)
    wo = P.tile([128, 8, D], BF16, "or_wo")
    wsrc = B.inp("w_o", [DEPTH, D, D], F32)
    for h in range(2):
        P.dma("pool", wo[:, :, h * 512:(h + 1) * 512],
              wsrc.v(wsrc.ap[l, :, h * 512:(h + 1) * 512].rearrange("(k p) j -> p k j", p=128)), disjoint=(h == 1))
    xt = [P.tile([128, D], F32, "or_x%d" % i) for i in range(2)]
    tmp = [P.tile([128, 512], F32, "or_t%d" % i) for i in range(2)]
    for c in range(st.NT):
        x = xt[c % 2]
        xd = T(Xd.ap[c * 128:(c + 1) * 128, :], Xdb[c])
        P.dma("sp", x, xd)
        sl = (c * 128) // st.SL
        for h in range(2):
            po = P.bank()
            P.mm(po, [(T(M.merged.ap[:, k, c * 128:(c + 1) * 128], M.mergedb[k][sl]), wo[:, k, h * 512:(h + 1) * 512]) for k in range(8)])
            P.tt(tmp[h], po, bc[:, h * 512:(h + 1) * 512], ALU.mult)
            P.tt(x[:, h * 512:(h + 1) * 512], x[:, h * 512:(h + 1) * 512], tmp[h], ALU.add, E=PE_ALT)
        P.dma("sp", xd, x)
    P.release(m0)


class MoeStream:
    pass


def moe_prepare(B, C, l, st, Xd, Xdb, modT, sidx, g2, wr):
    P = B.P
    L, NT, SL, NS = st.L, st.NT, st.SL, st.NS
    cap = 2 * L // NE
    S = MoeStream()
    S.st, S.cap = st, cap
    S.nsc = max(1, cap // 128)
    S.slots = min(cap, 128)
    nm = st.name
    S.X = P.tile([128, NT, D], F32, "moeX" + nm)
    S.Xb = [Buf("moeX%s%d" % (nm, c), "moeX" + nm) for c in range(NT)]
    S.h2b = P.tile([128, NT, D], BF16, "moeh2" + nm)
    S.afft = P.tile([128, NT, NE], F32, "moeaff" + nm)
    S.affhl = P.tile([128, NT, NE, 2], BF16, "moeaffhl" + nm)
    S.post = P.tile([128, NT, NE], F32, "moepos" + nm)
    S.posb = P.tile([16, L], BF16, "moeposb" + nm)
    S.bc5 = P.tile([128, D], F32, "moebc5" + nm)
    M = MixCtx()
    M.P, M.C = P, C
    bcast_cols(M, S.bc5, modT.v(modT.ap[:, 40:48, sidx]))
    m0 = P.mark()
    geff = P.tile([128, 8], F32, "moegeff")
    P.stt(geff, modT.v(modT.ap[:, 32:40, sidx]), 1.0, g2, ALU.add, ALU.mult)
    gbc = P.tile([128, D], F32, "moegbc")
    sbc = P.tile([128, D], F32, "moesbc")
    bcast_cols(M, gbc, geff)
    bcast_cols(M, sbc, modT.v(modT.ap[:, 24:32, sidx]))
    junk = P.tile([128, D], F32, "moejunk")
    h2f = [P.tile([128, D], F32, "moeh2f%d" % i) for i in range(2)]
    h2T = [P.tile([128, 8, 128], F32, "moeh2T%d" % i) for i in range(2)]
    ss = [P.tile([128, 1], F32, "moess%d" % i) for i in range(2)]
    sq = [P.tile([128, 1], F32, "moesq%d" % i) for i in range(2)]
    rs = [P.tile([128, 1], F32, "moers%d" % i) for i in range(2)]
    mx = [P.tile([128, 1], F32, "moemx%d" % i) for i in range(2)]
    sm = [P.tile([128, 1], F32, "moesm%d" % i) for i in range(2)]
    ex = [P.tile([128, NE], F32, "moeex%d" % i) for i in range(2)]
    idf = C["ident_f"]
    for c in range(NT):
        i = c % 2
        xc = T(S.X.ap[:, c, :], S.Xb[c])
        P.dma("sp", xc, T(Xd.ap[c * 128:(c + 1) * 128, :], Xdb[c]))
        P.act(junk, xc, AF.Square, accum=ss[i])
        P.act(sq[i], ss[i], AF.Sqrt, bias=C["eps"], scale=1.0 / D)
        P.recip(rs[i], sq[i])
        P.stt(h2f[i], xc, rs[i], gbc, ALU.mult, ALU.mult)
        P.tt(h2f[i], h2f[i], sbc, ALU.add)
        P.act(S.h2b[:, c, :], h2f[i], AF.Copy, dj=(c > 0))
        for hh_ in range(2):
            ps = P.bank()
            h2fi = h2f[i]

            def tr(pe, ps=ps, hh_=hh_, h2fi=h2fi):
                ins = None
                for kk in range(4):
                    k = hh_ * 4 + kk
                    ins = pe.transpose(ps.ap[:, kk * 128:(kk + 1) * 128], h2fi.ap[:, k * 128:(k + 1) * 128], idf.ap)
                return ins
            P.op("pe", tr, [h2fi, idf], [ps])
            P.copy(h2T[i].v(h2T[i].ap[:, hh_ * 4:(hh_ + 1) * 4, :].rearrange("p a b -> p (a b)")), ps, E="act")
        pl = P.bank()
        P.mm(pl[:, 0:NE], [(h2T[i][:, k, :], wr[:, k, :]) for k in range(8)])
        P.op("dve", lambda e, i=i, pl=pl: e.tensor_reduce(mx[i].ap, pl.ap[:, 0:NE], mybir.AxisListType.X, ALU.max, negate=True), [pl], [mx[i]])
        P.act(ex[i], pl[:, 0:NE], AF.Exp, bias=mx[i], accum=sm[i])
        P.recip(sm[i], sm[i])
        P.ts(S.afft[:, c, :], ex[i], sm[i], None, ALU.mult, dj=(c > 0))
    P.release(m0)
    B.dbg("aff_%s%d" % (nm, l), S.afft)
    m0 = P.mark()
    tmpf = P.tile([128, NT, NE], F32, "moetmpf")
    P.copy(S.affhl.v(S.affhl.ap[:, :, :, 0]), S.afft)
    P.copy(tmpf, S.affhl.v(S.affhl.ap[:, :, :, 0]))
    P.tt(tmpf, S.afft, tmpf, ALU.subtract)
    P.copy(S.affhl.v(S.affhl.ap[:, :, :, 1]), tmpf)
    affT = P.tile([16, L], F32, "moeaffT")
    work = P.tile([16, L], F32, "moework")
    ones = P.tile([16, L], F32, "moeones")
    P.memset(ones, 1.0)
    gs = min(4, NT)
    for cq in range(NT // gs):
        ps = P.bank()

        def tr2(pe, ps=ps, cq=cq):
            ins = None
            for cc in range(gs):
                c = cq * gs + cc
                ins = pe.transpose(ps.ap[0:16, cc * 128:(cc + 1) * 128], S.afft.ap[:, c, :], idf.ap)
            return ins
        P.op("pe", tr2, [S.afft, idf], [ps])
        P.copy(affT[:, cq * gs * 128:(cq + 1) * gs * 128], ps[0:16, 0:gs * 128], E="act")
    P.copy(work, affT)
    m8 = P.tile([16, 8], F32, "moem8")
    for r in range(cap // 8):
        P.op("dve", lambda e: e.max(m8.ap, work.ap), [work], [m8])
        if r < cap // 8 - 1:
            P.op("dve", lambda e: e.match_replace(work.ap, m8.ap, work.ap, -1.0), [work, m8], [work])
    mask = work
    P.ts(mask, affT, m8[:, 7:8], None, ALU.is_ge)
    incl = P.tile([16, L], F32, "moeincl")
    P.scan(incl, ones, mask, 0.0)
    P.tt(incl, incl, mask, ALU.mult)
    P.ts(incl, incl, -1.0, None, ALU.add)
    P.copy(S.posb, incl)
    B.dbg("posm_%s%d" % (nm, l), incl)
    for cq in range(NT // gs):
        ps = P.bank()

        def tr3(pe, ps=ps, cq=cq):
            ins = None
            for cc in range(gs):
                c = cq * gs + cc
                ins = pe.transpose(ps.ap[:, cc * NE:(cc + 1) * NE], incl.ap[0:16, c * 128:(c + 1) * 128], idf.ap[0:16, 0:16])
            return ins
        P.op("pe", tr3, [incl, idf], [ps])
        P.copy(S.post.v(S.post.ap[:, cq * gs:(cq + 1) * gs, :].rearrange("p a b -> p (a b)")), ps[:, 0:gs * NE], E="act")
    P.release(m0)
    return S


def moe_experts(B, C, l, streams):
    P = B.P
    m0 = P.mark()
    iota = P.tile([128, 256], F32, "moeiota", sg="cols")
    P.dma("sp", iota, B.inp("c_iota", [128, 256], F32))
    pidx = P.tile([128, 2], F32, "moepidx", sg="cols")
    P.dma("sp", pidx, B.inp("c_pidx2", [128, 2], F32))
    selt = [P.tile([16, 128], BF16, "moeselt%d" % i) for i in range(2)]
    for S in streams:
        nm, st = S.st.name, S.st
        S.S = P.tile([128, st.NT, S.cap], BF16, "moeS" + nm)
        S.xsT = P.tile([128, 8, S.cap], BF16, "moexsT" + nm)
        S.hidT = P.tile([128, NFC, S.cap], BF16, "moehid" + nm)
        S.hidTb = [Buf("moehid%s%d" % (nm, i)) for i in range(NFC // 2)]
        if S.nsc == 1:
            S.yacc = P.tile([128, D], F32, "moeyacc" + nm)
        S.ysg = P.tile([128, S.nsc, D], BF16, "moeysg" + nm)
        S.gcol = P.tile([128, 2, 2], F32, "moegcol" + nm)
        S.gtmp = P.tile([128, 2], F32, "moegtmp" + nm)
        S.sg = [P.tile([128, S.cap], F32, "moesg%s%d" % (nm, i)) for i in range(2)]
    st_words = sum(S.nsc * S.st.L // 2 for S in streams)
    free = P.AW - P.top - 64 - st_words
    dbl = free - 7 * 1024 >= st_words + 64
    for S in streams:
        S.STs = [P.tile([128, S.nsc, S.st.L], BF16, "moeST%s%d" % (S.st.name, i)) for i in range(2 if dbl else 1)]
    free_slots = (P.AW - P.top - 64) // 1024
    if False:
        UPW = 512
        n_dn = 3 if free_slots < 12 else 4
        n_up = max(4, min(6, 2 * ((free_slots - n_dn) // 4)))
    else:
        UPW = 256
        n_up = max(4, min(10, 2 * int(free_slots * 0.6 / 2)))
        n_dn = max(3, min(6, free_slots - n_up))
    upring = Ring(P, n_up, [128, 8, UPW], BF16, "moeup")
    dnring = Ring(P, n_dn, [128, 2, D], BF16, "moedn")
    up_pieces = [(c0, min(UPW, DEXP - c0)) for c0 in range(0, DEXP, UPW)]
    P.peaks["moe_cfg%d" % l] = (dbl, n_up, n_dn, free_slots)
    wg_src = B.inp("w_exp_gate", [DEPTH, NE, D, DEXP], F32)
    wu_src = B.inp("w_exp_up", [DEPTH, NE, D, DEXP], F32)
    wd_src = B.inp("w_exp_down", [DEPTH, NE, DEXP, D], F32)

    def build_S(e):
        for S in streams:
            for c in range(S.st.NT):
                P.ts(S.S[:, c, :], iota[:, 0:S.cap], S.post[:, c, e:e + 1], None, ALU.is_equal, dj=(c > 0))

    def build_gate(e):
        for S in streams:
            slots = S.slots
            for sc in range(S.nsc):
                pg = P.bank()
                P.mm(pg[0:slots, 0:2], [(S.S[:, c, sc * 128:sc * 128 + slots], S.affhl[:, c, e, :]) for c in range(S.st.NT)])
                P.copy(S.gtmp[0:slots, 0:2], pg[0:slots, 0:2])
                P.tt(S.gcol[0:slots, e % 2, sc:sc + 1], S.gtmp[0:slots, 0:1], S.gtmp[0:slots, 1:2], ALU.add)

    def build_ST(e):
        sl_ = selt[e % 2]
        P.ts(sl_, C["ones_bf"][0:16, :], C["ident_f"][0:16, e:e + 1], None, ALU.mult)
        for S in streams:
            st, slots = S.st, S.slots
            STt = S.STs[e % len(S.STs)]
            for n in range(st.NS):
                pb_ = P.bank()
                P.mm(pb_[0:slots, 0:st.SL], [(sl_[:, 0:slots], S.posb[:, n * st.SL:(n + 1) * st.SL])])
                for sc in range(S.nsc):
                    P.ts(STt[0:slots, sc, n * st.SL:(n + 1) * st.SL], pb_[0:slots, 0:st.SL], pidx[0:slots, sc:sc + 1], None, ALU.is_equal, dj=(n > 0 or sc > 0))

    build_S(0)
    build_gate(0)
    build_ST(0)
    for e in range(NE):
        for S in streams:
            st = S.st
            for k in range(8):
                px = P.bank()
                P.mm(px[:, 0:S.cap], [(S.h2b[:, c, k * 128:(k + 1) * 128], S.S[:, c, :]) for c in range(st.NT)])
                P.act(S.xsT[:, k, :], px[:, 0:S.cap], AF.Copy, dj=(k > 0))
        if e + 1 < NE:
            build_S(e + 1)
        lat_streams = [S for S in streams if S.nsc > 1]
        small_streams = [S for S in streams if S.nsc == 1]
        accs = {}
        for S in lat_streams:
            accs[id(S)] = [[P.bank_reserve() for h in range(2)] for sc in range(S.nsc)]
        wds = {}

        def down_piece(p_):
            wd = wds.pop(p_)
            for S in lat_streams:
                for sc in range(S.nsc):
                    for h in range(2):
                        P.mm_acc(accs[id(S)][sc][h][0:S.slots, :],
                                 [(T(S.hidT.ap[:, p_ * 2 + mm_, sc * 128:sc * 128 + S.slots], S.hidTb[p_]), wd[:, mm_, h * 512:(h + 1) * 512]) for mm_ in range(2)],
                                 start=(p_ == 0), stop=(p_ == NFC // 2 - 1))
            for S in small_streams:
                slots = S.slots
                for h in range(2):
                    po = P.bank()
                    P.mm(po[0:slots, :], [(T(S.hidT.ap[:, p_ * 2 + mm_, 0:slots], S.hidTb[p_]), wd[:, mm_, h * 512:(h + 1) * 512]) for mm_ in range(2)])
                    ya = S.yacc[0:slots, h * 512:(h + 1) * 512]
                    if p_ == 0:
                        P.copy(ya, po[0:slots, :])
                    else:
                        P.tt(ya, ya, po[0:slots, :], ALU.add)

        for pi_, (c0, ncol) in enumerate(up_pieces):
            wg = upring.next()
            P.dma("pool", wg[:, :, 0:ncol], wg_src.v(wg_src.ap[l, e, :, c0:c0 + ncol].rearrange("(k p) j -> p k j", p=128)))
            wu = upring.next()
            P.dma("pool", wu[:, :, 0:ncol], wu_src.v(wu_src.ap[l, e, :, c0:c0 + ncol].rearrange("(k p) j -> p k j", p=128)))
            wd = dnring.next()
            P.dma("pool", wd, wd_src.v(wd_src.ap[l, e, pi_ * 256:(pi_ + 1) * 256, :].rearrange("(m p) j -> p m j", p=128)))
            wds[pi_] = wd
            for mm_ in range(ncol // 128):
                m = c0 // 128 + mm_
                for S in streams:
                    cap = S.cap
                    pg = P.bank()
                    P.mm(pg[:, 0:cap], [(wg[:, k, mm_ * 128:(mm_ + 1) * 128], S.xsT[:, k, :]) for k in range(8)])
                    pu = P.bank()
                    P.mm(pu[:, 0:cap], [(wu[:, k, mm_ * 128:(mm_ + 1) * 128], S.xsT[:, k, :]) for k in range(8)])
                    sgi = S.sg[m % 2]
                    P.act(sgi, pg[:, 0:cap], AF.Silu)
                    P.tt(T(S.hidT.ap[:, m, :], S.hidTb[m // 2]), pu[:, 0:cap], sgi, ALU.mult, dj=(m % 2 == 1))
            if pi_ >= 1:
                down_piece(pi_ - 1)
        down_piece(NFC // 2 - 1)
        if e + 1 < NE:
            build_gate(e + 1)
            if dbl:
                build_ST(e + 1)
        for S in lat_streams:
            slots = S.slots
            for sc in range(S.nsc):
                for h in range(2):
                    a = accs[id(S)][sc][h]
                    P.stt(S.ysg[0:slots, sc, h * 512:(h + 1) * 512], a[0:slots, :], S.gcol[0:slots, e % 2, sc:sc + 1],
                          S.bc5[0:slots, h * 512:(h + 1) * 512], ALU.mult, ALU.mult)
                    P.bank_free(a)
        for S in small_streams:
            slots = S.slots
            for h in range(2):
                P.stt(S.ysg[0:slots, 0, h * 512:(h + 1) * 512], S.yacc[0:slots, h * 512:(h + 1) * 512], S.gcol[0:slots, e % 2, 0:1],
                      S.bc5[0:slots, h * 512:(h + 1) * 512], ALU.mult, ALU.mult)
        for S in streams:
            st, slots = S.st, S.slots
            for c in range(st.NT):
                for h in range(2):
                    po = P.bank()
                    STt = S.STs[e % len(S.STs)]
                    P.mm(po, [(STt[0:slots, sc, c * 128:(c + 1) * 128], S.ysg[0:slots, sc, h * 512:(h + 1) * 512]) for sc in range(S.nsc)])
                    xc = T(S.X.ap[:, c, h * 512:(h + 1) * 512], S.Xb[c])
                    P.tt(xc, xc, po, ALU.add)
        if e + 1 < NE and not dbl:
            build_ST(e + 1)
    P.release(m0)


def build_program(dbg=(), stop=None):
    B = Builder(dbg, stop)
    P = B.P
    C = build_consts(B)
    lat = Stream("lat", SEQ)
    ctx = Stream("ctx", CTXL)
    Xd = B.scratch("Xd", [SEQ, D], F32)
    Xdb = [Buf("Xd%d" % c, "Xd") for c in range(lat.NT)]
    XCd = B.scratch("XCd", [CTXL, D], F32)
    XCdb = [Buf("XCd%d" % c, "XCd") for c in range(ctx.NT)]
    xin = B.inp("x", [SEQ, D], F32)
    pos = B.inp("c_pos", [SEQ, D], F32)
    cin = B.inp("ctx", [CTXL, D], F32)
    m = P.mark()
    xtmp = [P.tile([128, D], F32, "xtmp%d" % i) for i in range(4)]
    ptmp = [P.tile([128, D], F32, "ptmp%d" % i) for i in range(4)]
    def pl(c):
        if c < lat.NT:
            P.dma("sp", xtmp[c % 4], xin.v(xin.ap[c * 128:(c + 1) * 128, :]))
            P.dma("sp", ptmp[c % 4], pos.v(pos.ap[c * 128:(c + 1) * 128, :]))
    for c in range(3):
        pl(c)
    for c in range(lat.NT):
        pl(c + 3)
        P.tt(xtmp[c % 4], xtmp[c % 4], ptmp[c % 4], ALU.add)
        P.dma("act", T(Xd.ap[c * 128:(c + 1) * 128, :], Xdb[c]), xtmp[c % 4])
    for c in range(ctx.NT):
        P.dma("sp", ptmp[c % 2], cin.v(cin.ap[c * 128:(c + 1) * 128, :]))
        P.dma("sp", T(XCd.ap[c * 128:(c + 1) * 128, :], XCdb[c]), ptmp[c % 2])
    P.release(m)
    Ks = {}
    for st in (ctx, lat):
        Ks[st.name] = (B.scratch("Kscr_" + st.name, [D // CG, st.NT, 128, 2, 2 * CG], F32), [Buf("K%s%d" % (st.name, g), "K" + st.name) for g in range(D // CG)])
    if stop is None:
        for st in (ctx, lat):
            filter_phase(B, C, 0, st, Ks[st.name][0], Ks[st.name][1])
        filt0_done = True
    else:
        filt0_done = False
    mods = adaln(B, C)
    stF = P.tile([128, 8], F32, "stF")
    stB = P.tile([128, 8], F32, "stB")
    win = B.inp("w_in", [DEPTH, D, DIN], F32)
    for l in range(DEPTH):
        last = l == DEPTH - 1
        mL = P.mark()
        for st in ((lat,) if last else (ctx, lat)):
            if l == 0 and filt0_done:
                continue
            filter_phase(B, C, l, st, Ks[st.name][0], Ks[st.name][1])
            P.peak("filter_" + st.name)
            if stop == "filter_" + st.name:
                kt = P.tile([128, 2, 2 * CG], F32, "kdbg")
                for (g_, f_) in ((0, 0), (3, st.NT - 1)):
                    P.dma("sp", kt, T(Ks[st.name][0].ap[g_, f_], Ks[st.name][1][g_]))
                    B.dbg("K_%d_%d" % (g_, f_), kt)
                return B.finish([]) or B
        g1 = load_cols(B, "norm1_g_c%d" % l, 8)
        geff = P.tile([128, 8, 2], F32, "geff1")
        for s in range(2):
            P.stt(geff.v(geff.ap[:, :, s]), mods[l].v(mods[l].ap[:, 8:16, s]), 1.0, g1, ALU.add, ALU.mult)
        for st, Xs, Xsb, sidx in ((ctx, XCd, XCdb, 1), (lat, Xd, Xdb, 0)):
            mS = P.mark()
            M = MixCtx()
            M.P, M.st, M.C, M.B, M.l, M.win = P, st, C, B, l, win
            M.Ks = Ks[st.name]
            M.stF, M.stB = stF, stB
            M.hT, M.hTb = hT_alloc(P, st, "hT")
            norm_to_hT(B, C, st, Xs, Xsb, geff.v(geff.ap[:, :, sidx]), mods[l].v(mods[l].ap[:, 0:8, sidx]), M.hT, M.hTb)
            M.dgring = Ring(P, 8, [128, 128], BF16, "diag")
            tag = "%s%d" % (st.name, l)
            if st is ctx and last:
                lru_path(M, only_states=True)
                P.release(mS)
                continue
            M.yT = P.tile([128, 8, st.L], BF16, "yT")
            M.yTb = [Buf("yT%d" % k) for k in range(8)]
            M.merged = P.tile([128, 8, st.L], BF16, "merged")
            M.mergedb = [[Buf("mg%d_%d" % (k, s)) for s in range(st.NS)] for k in range(8)]
            yTall = T(M.yT.ap, M.yTb)
            P.peak("pre")
            hyena_path(M)
            P.peak("hyena_" + st.name)
            B.dbg("ya_" + tag, yTall)
            if stop == "hyena_" + tag:
                return B.finish([]) or B
            merge_path(M, 0, "w_hy_out")
            if stop == "merge0_" + tag:
                B.dbg("merged_" + tag, T(M.merged.ap, [b for r in M.mergedb for b in r]))
                return B.finish([]) or B
            lru_path(M)
            P.peak("lru_" + st.name)
            B.dbg("yb_" + tag, yTall)
            if stop == "lru_" + tag:
                return B.finish([]) or B
            merge_path(M, 1, "w_lru_out")
            sconv_path(M)
            B.dbg("yc_" + tag, yTall)
            merge_path(M, 2, "w_sc_out")
            B.dbg("merged_" + tag, T(M.merged.ap, [b for r in M.mergedb for b in r]))
            if stop == "merged_" + tag:
                return B.finish([]) or B
            out_residual(M, Xs, Xsb, mods[l].v(mods[l].ap[:, 16:24, sidx]))
            if stop == "mix_" + tag:
                xo = P.tile([128, st.NT, D], F32, "xdbg")
                for c in range(st.NT):
                    P.dma("sp", xo[:, c, :], T(Xs.ap[c * 128:(c + 1) * 128, :], Xsb[c]), disjoint=(c > 0))
                B.dbg("xmix_" + tag, xo)
                return B.finish([]) or B
            P.release(mS)
        g2 = load_cols(B, "norm2_g_c%d" % l, 8)
        wr = P.tile([128, 8, NE], F32, "wrouter")
        wrs = B.inp("w_router", [DEPTH, D, NE], F32)
        P.dma("sp", wr, wrs.v(wrs.ap[l].rearrange("(k p) e -> p k e", p=128)))
        streams = []
        if not last:
            streams.append(moe_prepare(B, C, l, ctx, XCd, XCdb, mods[l], 1, g2, wr))
        streams.append(moe_prepare(B, C, l, lat, Xd, Xdb, mods[l], 0, g2, wr))
        if stop == "moeprep%d" % l:
            return B.finish([]) or B
        P.peak("moeprep%d" % l)
        moe_experts(B, C, l, streams)
        P.peak("moe%d" % l)
        for S in streams:
            st = S.st
            Xs, Xsb = (XCd, XCdb) if st is ctx else (Xd, Xdb)
            B.dbg("xmoe_%s%d" % (st.name, l), T(S.X.ap, S.Xb))
            if st is lat and last:
                break
            for c in range(st.NT):
                P.dma("sp", T(Xs.ap[c * 128:(c + 1) * 128, :], Xsb[c]), T(S.X.ap[:, c, :], S.Xb[c]))
        if stop == "moe%d" % l:
            return B.finish([]) or B
        if last:
            S = streams[-1]
            out = B.nc.dram_tensor("out", [SEQ, D], F32, kind="ExternalOutput").ap()
            outT = T(out, Buf("out"))
            gf = load_cols(B, "final_norm_g_cx", 8)
            gfbc = P.tile([128, D], F32, "gfbc")
            Mx = MixCtx()
            Mx.P, Mx.C = P, C
            bcast_cols(Mx, gfbc, gf)
            junk = P.tile([128, D], F32, "fjunk")
            ot = [P.tile([128, D], F32, "fo%d" % i) for i in range(2)]
            ss = [P.tile([128, 1], F32, "fss%d" % i) for i in range(2)]
            for c in range(lat.NT):
                i = c % 2
                xc = T(S.X.ap[:, c, :], S.Xb[c])
                P.act(junk, xc, AF.Square, accum=ss[i])
                P.act(ss[i], ss[i], AF.Sqrt, bias=C["eps"], scale=1.0 / D)
                P.recip(ss[i], ss[i])
                P.stt(ot[i], xc, ss[i], gfbc, ALU.mult, ALU.mult)
                P.dma("sp", outT.v(out[c * 128:(c + 1) * 128, :]), ot[i], disjoint=(c > 0))
            B.finish([outT])
            return B
        P.release(mL)
    B.finish([])
    return B


_CONST_CACHE = {}


def _const(name):
    if name in _CONST_CACHE:
        return _CONST_CACHE[name]
    if name == "c_ident_bf":
        v = bf(np.eye(128))
    elif name == "c_ident_f":
        v = np.eye(128, dtype=np.float32)
    elif name == "c_pos":
        v = grid_pos_embed(SEQ // 64)
    elif name.startswith("c_fwd") or name.startswith("c_inv"):
        L = int(name[5:])
        F, I = dft_consts(L)
        _CONST_CACHE["c_fwd%d" % L] = F
        _CONST_CACHE["c_inv%d" % L] = I
        return _CONST_CACHE[name]
    elif name.startswith("c_feats") or name.startswith("c_decay"):
        L = int(name[7:])
        fT, dec = filt_consts(L)
        _CONST_CACHE["c_feats%d" % L] = fT
        _CONST_CACHE["c_decay%d" % L] = dec
        return _CONST_CACHE[name]
    elif name == "c_iota":
        v = np.broadcast_to(np.arange(256, dtype=np.float32)[None, :], (128, 256)).copy()
    elif name == "c_pidx2":
        v = np.stack([np.arange(128, dtype=np.float32), np.arange(128, dtype=np.float32) + 128], axis=1).copy()
    elif name == "c_sel":
        v = np.zeros((16, NE, 128), np.float32)
        for e_ in range(NE):
            v[e_, e_, :] = 1.0
        v = bf(v)
    elif name == "c_pidx":
        v = np.arange(128, dtype=np.float32).reshape(128, 1).copy()
    else:
        raise KeyError(name)
    _CONST_CACHE[name] = v
    return v


def host_arrays(inputs, b, needed):
    out = {}
    for name in needed:
        if name.startswith("c_"):
            out[name] = _const(name)
        elif name == "x":
            out[name] = np.ascontiguousarray(inputs["x"][b])
        elif name == "ctx":
            out[name] = np.ascontiguousarray(inputs["ctx"][b])
        elif name == "cvec":
            out[name] = np.concatenate([col(inputs["c"][b]), col(inputs["c_ctx"])], axis=1)
        elif name in inputs:
            out[name] = np.ascontiguousarray(inputs[name])
        elif name == "final_norm_g_cx":
            out[name] = col(inputs["final_norm_g"])
        elif name.startswith("filtcols_"):
            l = int(name.split("_")[1])
            out[name] = np.ascontiguousarray(np.stack([inputs["hy_filt_b1"][l], inputs["hy_filt_b2"][l], inputs["hy_filt_freq"][l]], axis=1).astype(np.float32))
        else:
            base, l = name.rsplit("_c", 1)
            l = int(l)
            colsrc = {"b_ada": inputs["b_ada"], "norm1_g": inputs["norm1_g"], "norm2_g": inputs["norm2_g"],
                      "hy_conv_w": inputs["hy_conv_w"], "hy_conv_b": inputs["hy_conv_b"],
                      "lru_conv_w": inputs["lru_conv_w"], "lru_conv_b": inputs["lru_conv_b"],
                      "lru_ba": inputs["lru_ba"], "lru_bx": inputs["lru_bx"], "lru_lambda": inputs["lru_lambda"],
                      "sc_conv_w": inputs["sc_conv_w"]}
            out[name] = col(np.asarray(colsrc[base][l]).reshape(-1))
    return out


TWO_PI = 2.0 * math.pi


def sin_reduced(P, out, arg, tmp_i, tmp_f):
    P.ts(tmp_f, arg, 1.0 / TWO_PI, None, ALU.mult)
    P.copy(tmp_i, tmp_f)
    P.copy(tmp_f, tmp_i)
    P.stt(arg, tmp_f, -TWO_PI, arg, ALU.mult, ALU.add)
    P.ts(tmp_f, arg, math.pi, TWO_PI, ALU.is_gt, ALU.mult)
    P.tt(arg, arg, tmp_f, ALU.subtract)
    P.ts(tmp_f, arg, -math.pi, TWO_PI, ALU.is_lt, ALU.mult)
    P.tt(arg, arg, tmp_f, ALU.add)
    P.ts(arg, arg, math.pi, -math.pi, ALU.min, ALU.max)
    P.act(out, arg, AF.Sin)


def filter_phase(B, C, l, st, Kscr, Kb):
    P = B.P
    L, NT = st.L, st.NT
    SL, NS = st.SL, st.NS
    m0 = P.mark()
    h2 = P.tile([64, L], F32, "fh2")
    w3 = P.tile([64, 4096], F32, "fw3")
    P.dma("sp", w3, B.inp("hy_filt_w3", [DEPTH, 64, 4096]).v(B.inp("hy_filt_w3", [DEPTH, 64, 4096]).ap[l]))
    mt = P.mark()
    featsT = P.tile([33, L], F32, "featsT")
    P.dma("sp", featsT, B.inp("c_feats%d" % L, [33, L], F32))
    w1 = P.tile([33, 64], F32, "fw1")
    P.dma("sp", w1, B.inp("hy_filt_w1", [DEPTH, 33, 64]).v(B.inp("hy_filt_w1", [DEPTH, 33, 64]).ap[l]))
    w2 = P.tile([64, 64], F32, "fw2")
    P.dma("sp", w2, B.inp("hy_filt_w2", [DEPTH, 64, 64]).v(B.inp("hy_filt_w2", [DEPTH, 64, 64]).ap[l]))
    fc = P.tile([64, 3], F32, "fcols")
    P.dma("sp", fc, B.inp("filtcols_%d" % l, [64, 3], F32))
    fb = P.tile([64, 2], F32, "fb")
    P.tt(fb[:, 0:1], fc[:, 0:1], fc[:, 2:3], ALU.mult)
    P.tt(fb[:, 1:2], fc[:, 1:2], fc[:, 2:3], ALU.mult)
    arg = P.tile([64, L], F32, "farg")
    tf = P.tile([64, L], F32, "ftf")
    ti = P.tile([64, L], I32, "fti")
    h1 = P.tile([64, L], F32, "fh1")
    for (wmat, src, dst, bi) in ((w1, featsT, h1, 0), (w2, h1, h2, 1)):
        for s_ in range(NS):
            ps = P.bank()
            pv = ps[0:64, 0:SL]
            P.mm(pv, [(wmat, src[:, s_ * SL:(s_ + 1) * SL])])
            P.act(arg[:, s_ * SL:(s_ + 1) * SL], pv, AF.Identity, bias=fb[:, bi:bi + 1], scale=fc[:, 2:3])
        sin_reduced(P, dst, arg, ti, tf)
    P.release(mt)
    B.dbg("filt_h2_%s%d" % (st.name, l), h2)
    fwd_src = B.inp("c_fwd%d" % L, [NT, 128, 2, NT, 128], BF16)
    dec_src = B.inp("c_decay%d" % L, [128, NT, D], F32)
    hb_src = B.inp("hy_bias", [DEPTH, 2, D], F32)
    h2b = P.tile([64, L], BF16, "fh2b")
    P.copy(h2b, h2)
    w3b = P.tile([64, 4096], BF16, "fw3b")
    P.copy(w3b, w3, E="act")
    m1 = P.mark()
    NG = D // CG
    NFR = 4
    fring = [P.tile([128, 2, NT, 128], BF16, "ffwd%d" % i) for i in range(NFR)]
    kout = [P.tile([128, 2, 2 * CG], F32, "fkout%d" % i) for i in range(3)]
    hd = [P.tile([128, 2, 2, CG], F32, "fhd%d" % i) for i in range(3)]
    ab = [P.tile([128, 2, 2, CG], BF16, "fab%d" % i) for i in range(3)]
    G = []
    for pp in range(2):
        gs_ = MixCtx()
        gs_.dec = P.tile([128, NT, CG], F32, "fdec%d" % pp)
        gs_.Eg = P.tile([128, NT, 2 * CG], BF16, "fE%d" % pp)
        gs_.Og = P.tile([128, NT, 2 * CG], BF16, "fO%d" % pp)
        gs_.biasbc = P.tile([128, 2 * CG], F32, "fbias%d" % pp)
        gs_.rnorm = P.tile([128, 2 * CG], F32, "frnorm%d" % pp)
        G.append(gs_)

    def g_begin(g):
        gs_ = G[g % 2]
        P.dma("sp", gs_.dec, dec_src.v(dec_src.ap[:, :, g * CG:(g + 1) * CG]))
        for o in range(2):
            P.dma("sp", gs_.biasbc[:, o * CG:(o + 1) * CG],
                  hb_src.v(hb_src.ap[l, o:o + 1, g * CG:(g + 1) * CG].partition_broadcast(128)), disjoint=(o == 1))
        gs_.nps = P.bank_reserve()

    def g_stage1(g, c):
        gs_ = G[g % 2]
        i = c % 3
        for o in range(2):
            ps = P.bank()
            for dr in range(2):
                colo = (o * 2 + dr) * D + g * CG
                P.mm(ps[:, dr * CG:(dr + 1) * CG], [(h2b[:, c * 128:(c + 1) * 128], w3b[:, colo:colo + CG])])
            decb = gs_.dec.v(gs_.dec.ap[:, c, :].unsqueeze(1).broadcast_to([128, 2, CG]))
            P.tt(hd[i][:, o, :, :], ps.v(ps.ap.rearrange("p (a b) -> p a b", a=2)), decb, ALU.mult, dj=(o > 0))
        if c == 0:
            P.memset(hd[i][0:1, :, 1, :], 0.0)
        P.act(ab[i], hd[i], AF.Abs)
        Ev = gs_.Eg.v(gs_.Eg.ap[:, c, :].rearrange("p (a b) -> p a b", a=2))
        Ov = gs_.Og.v(gs_.Og.ap[:, c, :].rearrange("p (a b) -> p a b", a=2))
        P.tt(Ev, hd[i][:, :, 0, :], hd[i][:, :, 1, :], ALU.add, dj=(c > 0))
        P.tt(Ov, hd[i][:, :, 1, :], hd[i][:, :, 0, :], ALU.subtract, dj=(c > 0))

    def g_stage2(g, c):
        gs_ = G[g % 2]
        i = c % 3
        for o in range(2):
            P.mm_acc(gs_.nps[:, o * CG:(o + 1) * CG],
                     [(C["ones_bf"], ab[i][:, o, 0, :]), (C["ones_bf"], ab[i][:, o, 1, :])],
                     start=(c == 0 and o == 0), stop=(c == NT - 1 and o == 1))

    def g_end(g):
        gs_ = G[g % 2]
        P.recip(gs_.rnorm, gs_.nps)
        P.bank_free(gs_.nps)
        if g == 0:
            B.dbg("filt_rnorm_%s%d" % (st.name, l), gs_.rnorm)

    def pref(g, fi):
        if g < NG and fi < NT:
            P.dma("sp", fring[(g * NT + fi) % NFR], fwd_src.v(fwd_src.ap[fi]))

    def g_dft(g, fi):
        gs_ = G[g % 2]
        fw = fring[(g * NT + fi) % NFR]
        pr = P.bank()
        P.mm(pr, [(fw[:, 0, c, :], gs_.Eg[:, c, :]) for c in range(NT)])
        pi_ = P.bank()
        P.mm(pi_, [(fw[:, 1, c, :], gs_.Og[:, c, :]) for c in range(NT)])
        ko = kout[fi % 3]
        P.tt(ko[:, 0, :], pr, gs_.rnorm, ALU.mult)
        P.tt(ko[:, 0, :], ko[:, 0, :], gs_.biasbc, ALU.add)
        P.tt(ko[:, 1, :], pi_, gs_.rnorm, ALU.mult)
        P.dma("act", T(Kscr.ap[g, fi], Kb[g]), ko, disjoint=True)

    g_begin(0)
    for c in range(NT):
        g_stage1(0, c)
        g_stage2(0, c)
    g_end(0)
    for g in range(NG):
        for q in range(NFR - 1):
            pref(g, q)
        if g + 1 < NG:
            g_begin(g + 1)
        for t_ in range(NT):
            pref(g, t_ + NFR - 1)
            if g + 1 < NG:
                g_stage1(g + 1, t_)
            g_dft(g, t_)
            if g + 1 < NG:
                g_stage2(g + 1, t_)
        if g + 1 < NG:
            g_end(g + 1)
    P.release(m0)


_PROGRAM = [None]


def _get_program():
    if _PROGRAM[0] is None:
        _PROGRAM[0] = build_program()
    return _PROGRAM[0]


def kernel(**inputs):
    inputs = {k: np.asarray(v) for k, v in inputs.items()}
    B = _get_program()
    names = list(B.inputs.keys())
    shared = {}
    in_maps = []
    nb = inputs["x"].shape[0]
    for b in range(nb):
        per_core = {"x", "ctx", "cvec"}
        need_b = set(n for n in names if n in per_core or n not in shared)
        h = host_arrays(inputs, b, need_b)
        for n in names:
            if n not in per_core and n not in shared:
                shared[n] = h[n]
        in_maps.append({n: (h[n] if n in per_core else shared[n]) for n in names})
    res = run_bass_kernel_spmd(B.nc, in_maps, core_ids=list(range(nb)))
    out = np.stack([np.asarray(res.results[b]["out"], dtype=np.float32) for b in range(nb)], axis=0)
    return out
```

```python
import math
import numpy as np
import ml_dtypes
import concourse.bass as bass
import concourse.mybir as mybir
from concourse.bass_utils import run_bass_kernel_spmd

F32 = mybir.dt.float32
BF16 = mybir.dt.bfloat16
I32 = mybir.dt.int32
AF = mybir.ActivationFunctionType
ALU = mybir.AluOpType

D = 1024
SEQ = 2048
CTXL = 256
DEPTH = 2
NE = 16
DEXP = 2816
NFC = DEXP // 128
OFF_LRU_GATE = 3 * D
OFF_LRU_REC = OFF_LRU_GATE + D
OFF_SC = OFF_LRU_REC + D
OFF_GATE = OFF_SC + 3 * D
DIN = OFF_GATE + 3 * D
EPS = 1e-6
CG = 256
PE_ALT = "dve"


CUR_PROG = [None]


class Buf:
    __slots__ = ("name", "w", "r", "sem", "semval", "sg")

    def __init__(self, name="", sg=None):
        self.name = name
        self.sg = sg
        self.w = None
        self.r = dict(CUR_PROG[0].fence) if CUR_PROG[0] is not None else {}
        self.sem = None
        self.semval = 0


class T:
    __slots__ = ("ap", "bufs")

    def __init__(self, ap, bufs):
        self.ap = ap
        self.bufs = bufs if isinstance(bufs, (list, tuple)) else [bufs]

    def __getitem__(self, idx):
        return T(self.ap[idx], self.bufs)

    def v(self, ap):
        return T(ap, self.bufs)


def _ap(x):
    return x.ap if isinstance(x, T) else x


def _bufs(xs):
    out = []
    for x in xs:
        if isinstance(x, T):
            out.extend(x.bufs)
    return out


class Prog:
    def __init__(self):
        self.nc = bass.Bass("TRN2", target_bir_lowering=False)
        nc = self.nc
        self.fence = {}
        CUR_PROG[0] = None
        self.eng = {"pe": nc.tensor, "act": nc.scalar, "dve": nc.vector, "pool": nc.gpsimd, "sp": nc.sync}
        self.sem = {e: nc.alloc_semaphore("s_" + e) for e in ("pe", "act", "dve", "pool")}
        self.cnt = {e: 0 for e in self.sem}
        self.known = {e: {} for e in self.eng}
        self.vc = {}
        self.semreg = {}
        self.nbuf = 0
        self.AW = 52992
        self.arena = nc.alloc_sbuf_tensor("arena", [128, self.AW], F32).ap()
        self.top = 0
        self.hw = 0
        self.peaks = {}
        self.banks = []
        for i in range(8):
            ap = nc.alloc_psum_tensor("bank%d" % i, [128, 512], F32).ap()
            self.banks.append(T(ap, Buf("bank%d" % i)))
        self.bank_i = 0
        self.reserved = set()
        CUR_PROG[0] = self

    def alloc(self, words, name=""):
        words = (words + 7) // 8 * 8
        off = self.top
        self.top += words
        self.hw = max(self.hw, self.top)
        assert self.top <= self.AW, "SBUF arena overflow %s: %d > %d" % (name, self.top, self.AW)
        return off

    def tile(self, shape, dt=F32, name="", nb=None, sg=None):
        free = int(np.prod(shape[1:]))
        words = free if dt in (F32, I32) else (free + 1) // 2
        off = self.alloc(words, name)
        ap = self.arena[0:shape[0], off:off + words]
        if dt != F32:
            ap = ap.bitcast(dt)
            if ap.shape[1] != free:
                ap = ap[:, 0:free]
        if len(shape) == 3:
            ap = ap.rearrange("p (a b) -> p a b", b=shape[2])
        elif len(shape) == 4:
            ap = ap.rearrange("p (a b c) -> p a b c", b=shape[2], c=shape[3])
        return T(ap, nb if nb is not None else Buf(name, sg))

    def mark(self):
        return self.top

    def peak(self, name):
        self.peaks[name] = max(self.peaks.get(name, 0), self.hw)
        self.hw = self.top

    def release(self, m):
        self.top = m
        f = {}
        for e in self.cnt:
            if self.cnt[e] > 0:
                f[e] = (e, self.cnt[e])
        for nm, ent in self.semreg.items():
            if ent[1] > 0:
                f[nm] = ("dma", nm, ent[1], ent[0])
        self.fence = f

    def bank(self):
        while True:
            b = self.banks[self.bank_i]
            self.bank_i = (self.bank_i + 1) % 8
            if id(b) not in self.reserved:
                return b

    def bank_reserve(self):
        b = self.bank()
        self.reserved.add(id(b))
        return b

    def bank_free(self, b):
        self.reserved.discard(id(b))

    def _key(self, tok):
        return tok[0] if tok[0] != "dma" else tok[1]

    def _wait(self, E, toks):
        need = {}
        for tok in toks:
            if tok is None:
                continue
            k = self._key(tok)
            val = self.semreg[k][1] if tok[0] == "dma" else tok[1]
            if k not in need or need[k][0] < val:
                need[k] = (val, tok)
        kn = self.known[E]
        for k, (val, tok) in need.items():
            if kn.get(k, 0) >= val:
                continue
            semh = tok[3] if tok[0] == "dma" else self.sem[tok[0]]
            self.eng[E].wait_ge(semh, val)
            kn[k] = val
            snap = self.vc.get(tok[:3])
            if snap:
                for kk, vv in snap.items():
                    if kn.get(kk, 0) < vv:
                        kn[kk] = vv

    def _deps(self, reads, writes, E=None, dj=False):
        toks = []
        for b in reads:
            if b.w is not None:
                toks.append(b.w)
        for b in writes:
            if b.w is not None and not (dj and b.w[0] == E):
                toks.append(b.w)
            toks.extend(b.r.values())
        return toks

    def _commit(self, tok, E, reads, writes):
        self.vc[tok[:3]] = dict(self.known[E])
        k = self._key(tok)
        for b in reads:
            b.r[k] = tok
        for b in writes:
            b.w = tok
            b.r = {}

    def op(self, E, fn, reads, writes, dj=False):
        reads = _bufs(reads)
        writes = _bufs(writes)
        self._wait(E, self._deps(reads, writes, E, dj))
        ins = fn(self.eng[E])
        self.cnt[E] += 1
        ins.then_inc(self.sem[E], 1)
        tok = (E, self.cnt[E])
        self._commit(tok, E, reads, writes)
        return tok

    def dma(self, q, out, in_, sembuf=None, disjoint=False):
        reads = _bufs([in_])
        writes = _bufs([out])
        deps = self._deps(reads, writes)
        if disjoint:
            skip = set(id(b.w) for b in writes if b.w is not None and b.w[0] == "dma")
            deps = [t for t in deps if id(t) not in skip]
        self._wait(q, deps)
        sb = sembuf if sembuf is not None else writes[0]
        key = sb.sg or sb.name
        assert key, "dma target Buf needs a name"
        ent = self.semreg.get(key)
        if ent is None:
            ent = [self.nc.alloc_semaphore("d%d" % len(self.semreg)), 0]
            self.semreg[key] = ent
        ent[1] += 16
        self.eng[q].dma_start(out=_ap(out), in_=_ap(in_)).then_inc(ent[0], 16)
        tok = ("dma", key, ent[1], ent[0])
        self._commit(tok, q, reads, writes)
        return tok

    def mm(self, out, terms):
        reads = []
        for l, r in terms:
            reads += [l, r]
        n = len(terms)

        def fn(pe):
            ins = None
            for i, (l, r) in enumerate(terms):
                ins = pe.matmul(_ap(out), _ap(l), _ap(r), start=(i == 0), stop=(i == n - 1))
            return ins
        return self.op("pe", fn, reads, [out])

    def mm_acc(self, out, terms, start, stop):
        reads = []
        for l, r in terms:
            reads += [l, r]
        n = len(terms)

        def fn(pe):
            ins = None
            for i, (l, r) in enumerate(terms):
                ins = pe.matmul(_ap(out), _ap(l), _ap(r), start=(start and i == 0), stop=(stop and i == n - 1))
            return ins
        return self.op("pe", fn, reads, [out])

    def transpose(self, out, in_, ident):
        return self.op("pe", lambda pe: pe.transpose(_ap(out), _ap(in_), _ap(ident)), [in_, ident], [out])

    def act(self, out, in_, func, bias=None, scale=1.0, accum=None, dj=False):
        reads = [in_] + ([bias] if isinstance(bias, T) else []) + ([scale] if isinstance(scale, T) else [])
        writes = [out] + ([accum] if accum is not None else [])
        kw = {}
        if bias is not None:
            kw["bias"] = _ap(bias)
        if accum is not None:
            kw["accum_out"] = _ap(accum)
        return self.op("act", lambda e: e.activation(_ap(out), _ap(in_), func, scale=_ap(scale), **kw), reads, writes, dj=dj)

    def tt(self, out, a, b, op, E="dve", dj=False):
        return self.op(E, lambda e: e.tensor_tensor(_ap(out), _ap(a), _ap(b), op), [a, b], [out], dj=dj)

    def ts(self, out, a, s1, s2, op0, op1=None, E="dve", accum=None, dj=False):
        reads = [a] + [s for s in (s1, s2) if isinstance(s, T)]
        writes = [out] + ([accum] if accum is not None else [])
        if op1 is None:
            return self.op(E, lambda e: e.tensor_scalar(_ap(out), _ap(a), _ap(s1), None, op0), reads, writes, dj=dj)
        kw = {"accum_out": _ap(accum)} if accum is not None else {}
        return self.op(E, lambda e: e.tensor_scalar(_ap(out), _ap(a), _ap(s1), _ap(s2), op0, op1, **kw), reads, writes, dj=dj)

    def stt(self, out, a, s, b, op0, op1):
        reads = [a, b] + ([s] if isinstance(s, T) else [])
        return self.op("dve", lambda e: e.scalar_tensor_tensor(_ap(out), _ap(a), _ap(s), _ap(b), op0, op1), reads, [out])

    def copy(self, out, in_, E="dve"):
        if E == "act":
            return self.act(out, in_, AF.Copy)
        return self.op(E, lambda e: e.tensor_copy(_ap(out), _ap(in_)), [in_], [out])

    def memset(self, out, val, E="dve"):
        return self.op(E, lambda e: e.memset(_ap(out), val), [], [out])

    def scan(self, out, a, b, init):
        reads = [a, b] + ([init] if isinstance(init, T) else [])
        return self.op("dve", lambda e: e.tensor_tensor_scan(_ap(out), _ap(a), _ap(b), _ap(init), ALU.mult, ALU.add), reads, [out])

    def recip(self, out, in_):
        return self.op("dve", lambda e: e.reciprocal(_ap(out), _ap(in_)), [in_], [out])


def col(v):
    v = np.asarray(v, np.float32).reshape(-1, 128)
    return np.ascontiguousarray(v.T)


def bf(a):
    return np.ascontiguousarray(np.asarray(a, np.float32).astype(ml_dtypes.bfloat16))


def sincos_1d(pos, dim):
    half = dim // 2
    omega = 1.0 / (10000.0 ** (np.arange(half, dtype=np.float32) / np.float32(half)))
    ang = pos[:, None].astype(np.float32) * omega[None, :].astype(np.float32)
    return np.concatenate([np.sin(ang), np.cos(ang)], axis=-1).astype(np.float32)


def grid_pos_embed(rows, gw=64):
    half = D // 2
    er = sincos_1d(np.arange(rows, dtype=np.float32), half)
    ec = sincos_1d(np.arange(gw, dtype=np.float32), half)
    emb = np.concatenate([np.broadcast_to(er[:, None, :], (rows, gw, half)),
                          np.broadcast_to(ec[None, :, :], (rows, gw, half))], axis=-1)
    return emb.reshape(rows * gw, D).astype(np.float32)


def dft_consts(L):
    NTc = L // 128
    f = np.arange(L, dtype=np.int64)
    t = np.arange(L, dtype=np.int64)
    ph = ((2 * f[:, None] + 1) * t[None, :]) % (4 * L)
    ang = ph.astype(np.float64) * (2.0 * np.pi / (4 * L))
    C = np.cos(ang)
    S = np.sin(ang)
    def fwd(M):
        return M.reshape(NTc, 128, NTc, 128).transpose(0, 3, 2, 1)
    FWD = np.stack([fwd(C), fwd(S)], axis=2)
    INV = np.stack([C.reshape(NTc, 128, L) / L, -S.reshape(NTc, 128, L) / L], axis=2)
    return bf(FWD), bf(INV)


def filt_consts(L):
    bands = 16
    t01 = np.linspace(0.0, 1.0, L, dtype=np.float32)[:, None]
    w = (np.float32(2.0 * math.pi / L) * np.arange(L, dtype=np.float32))[:, None]
    fr = np.linspace(1e-4, bands - 1, bands, dtype=np.float32)[None, :]
    feats = np.concatenate([t01, np.cos(fr * w), -np.sin(fr * w)], axis=-1).astype(np.float32)
    mn = math.log(1e-2) / 1.5
    mx = math.log(1e-2) / 0.3
    deltas = np.linspace(mn, mx, D, dtype=np.float32)
    decay = np.exp(-t01 * np.abs(deltas)[None, :]).astype(np.float32)
    featsT = np.ascontiguousarray(feats.T)
    decay_l = np.ascontiguousarray(decay.reshape(L // 128, 128, D).transpose(1, 0, 2))
    return featsT, decay_l


class Stream:
    def __init__(self, name, L):
        self.name = name
        self.L = L
        self.NT = L // 128
        self.SL = min(512, L)
        self.NS = L // self.SL


class Builder:
    def __init__(self, dbg=(), stop=None):
        self.P = Prog()
        self.nc = self.P.nc
        self.inputs = {}
        self.dbg_req = set(dbg)
        self.dbg_out = {}
        self.stop = stop

    def inp(self, name, shape, dt=F32):
        if name not in self.inputs:
            ap = self.nc.dram_tensor(name, list(shape), dt, kind="ExternalInput").ap()
            self.inputs[name] = T(ap, Buf(name))
        return self.inputs[name]

    def scratch(self, name, shape, dt=F32):
        ap = self.nc.dram_tensor(name, list(shape), dt, kind="Internal").ap()
        return T(ap, Buf(name))

    def dbg(self, name, t, shape=None):
        if name not in self.dbg_req:
            return
        P = self.P
        shp = list(t.ap.shape)
        o = self.nc.dram_tensor("dbg_" + name, shp, t.ap.dtype, kind="ExternalOutput").ap()
        ob = T(o, Buf("dbg_" + name, "dbg"))
        P.dma("sp", ob, t)
        self.dbg_out[name] = ob

    def finish(self, outs):
        P = self.P
        toks = []
        for o in list(outs) + list(self.dbg_out.values()):
            for b in o.bufs:
                if b.w is not None:
                    toks.append(b.w)
        P._wait("sp", toks)


def build_consts(B):
    P = B.P
    c = {}
    c["ident_bf"] = P.tile([128, 128], BF16, "ident_bf", sg="cols")
    P.dma("sp", c["ident_bf"], B.inp("c_ident_bf", [128, 128], BF16))
    c["ident_f"] = P.tile([128, 128], F32, "ident_f", sg="cols")
    P.dma("sp", c["ident_f"], B.inp("c_ident_f", [128, 128], F32))
    c["eps"] = P.tile([128, 1], F32, "eps")
    P.memset(c["eps"], EPS)
    c["one"] = P.tile([128, 1], F32, "one")
    P.memset(c["one"], 1.0)
    c["ones_bf"] = P.tile([128, 128], BF16, "ones_bf")
    P.memset(c["ones_bf"], 1.0)
    c["ones_f"] = P.tile([128, 128], F32, "ones_f")
    P.memset(c["ones_f"], 1.0)
    return c


def load_cols(B, name, n, dt=F32):
    t = B.P.tile([128, n], dt, name, sg="cols")
    B.P.dma("sp", t, B.inp(name, [128, n], dt))
    return t


def adaln(B, C):
    P = B.P
    cc = load_cols(B, "cvec", 16)
    sil = P.tile([128, 8, 2], BF16, "silu_c")
    silf = P.tile([128, 16], F32, "silu_cf")
    P.act(silf, cc, AF.Silu)
    P.copy(sil.v(sil.ap[:, :, 0]), silf[:, 0:8])
    P.copy(sil.v(sil.ap[:, :, 1]), silf[:, 8:16])
    outs = [P.tile([128, 48, 2], F32, "mod%d" % l) for l in range(DEPTH)]
    bcols = [load_cols(B, "b_ada_c%d" % l, 48) for l in range(DEPTH)]
    m1 = P.mark()
    ring = [P.tile([128, 8, 512], BF16, "wada%d" % i) for i in range(3)]
    wsrc = B.inp("w_ada", [DEPTH, D, 6 * D], F32)
    ri = 0
    import os
    NL_ = int(os.environ.get("ADA_NL", DEPTH)); NJ_ = int(os.environ.get("ADA_NJ", 12))
    for l in range(NL_):
        for js in range(NJ_):
            w = ring[ri % 3]
            ri += 1
            src = wsrc.v(wsrc.ap[l, :, js * 512:(js + 1) * 512].rearrange("(k p) j -> p k j", p=128))
            P.dma("pool", w, src)
            for jj in range(4):
                j = js * 4 + jj
                ps = P.bank()
                pv = ps[:, 0:2]
                P.mm(pv, [(w[:, k, jj * 128:(jj + 1) * 128], sil[:, k, :]) for k in range(8)])
                P.act(outs[l][:, j, :], pv, AF.Identity, bias=bcols[l][:, j:j + 1], dj=(j > 0))
    P.release(m1)
    return outs


def norm_to_hT(B, C, st, Xd, Xdb, gcol, shiftcol, hT, hT_bufs):
    P = B.P
    m = P.mark()
    junk = P.tile([128, 1024], F32, "nrm_junk")
    xt = [P.tile([128, 1024], F32, "nrm_x%d" % i) for i in range(2)]
    xn = [P.tile([128, 1024], BF16, "nrm_xn%d" % i) for i in range(2)]
    ss = [P.tile([128, 1], F32, "nrm_ss%d" % i) for i in range(2)]
    sq = [P.tile([128, 1], F32, "nrm_sq%d" % i) for i in range(2)]
    rs = [P.tile([128, 1], F32, "nrm_rs%d" % i) for i in range(2)]
    idn = C["ident_bf"]
    for c in range(st.NT):
        i = c % 2
        P.dma("sp", xt[i], T(Xd.ap[c * 128:(c + 1) * 128, :], Xdb[c]))
        P.act(junk, xt[i], AF.Square, accum=ss[i])
        P.act(sq[i], ss[i], AF.Sqrt, bias=C["eps"], scale=1.0 / D)
        P.recip(rs[i], sq[i])
        P.ts(xn[i], xt[i], rs[i], None, ALU.mult)
        ps = P.bank()
        pb = ps.v(ps.ap.bitcast(BF16))
        xni = xn[i]

        def tr(pe, pb=pb, xni=xni):
            ins = None
            for k in range(8):
                ins = pe.transpose(pb.ap[:, k * 128:(k + 1) * 128], xni.ap[:, k * 128:(k + 1) * 128], idn.ap)
            return ins
        P.op("pe", tr, [xni, idn], [pb])
        sl = (c * 128) // st.SL
        for k in range(8):
            dst = T(hT.ap[:, k, c * 128:(c + 1) * 128], hT_bufs[k][c])
            if c % 2 == 0:
                P.act(dst, pb[:, k * 128:(k + 1) * 128], AF.Identity, bias=shiftcol[:, k:k + 1], scale=gcol[:, k:k + 1])
            else:
                P.ts(dst, pb[:, k * 128:(k + 1) * 128], gcol[:, k:k + 1], shiftcol[:, k:k + 1], ALU.mult, ALU.add)
    P.release(m)


def hT_alloc(P, st, name):
    hT = P.tile([128, 8, st.L], BF16, name)
    bufs = [[Buf("%s_%d_%d" % (name, k, c)) for c in range(st.NT)] for k in range(8)]
    return hT, bufs


def hT_slab(hT, bufs, k, s, st, lo=None, hi=None):
    cps = st.SL // 128
    return T(hT.ap[:, k, s * st.SL:(s + 1) * st.SL], bufs[k][s * cps:(s + 1) * cps])


class Ring:
    def __init__(self, P, n, shape, dt, name):
        self.tiles = [P.tile(shape, dt, "%s%d" % (name, i)) for i in range(n)]
        self.i = 0

    def next(self):
        t = self.tiles[self.i % len(self.tiles)]
        self.i += 1
        return t


class MixCtx:
    pass


def load_win(M, col0, ncol, ring):
    w = ring.next()
    src = M.win.v(M.win.ap[M.l, :, col0:col0 + ncol].rearrange("(k p) j -> p k j", p=128))
    M.P.dma("pool", w[:, :, 0:ncol], src)
    return w


def make_diag(M, out_bf, wcol):
    M.P.ts(out_bf, M.C["ident_f"], wcol, None, ALU.mult)


def hslab(M, k, s):
    st = M.st
    cps = st.SL // 128
    return T(M.hT.ap[:, k, s * st.SL:(s + 1) * st.SL], M.hTb[k][s * cps:(s + 1) * cps])


def yslab(M, k, s):
    st = M.st
    return T(M.yT.ap[:, k, s * st.SL:(s + 1) * st.SL], M.yTb[k])


def proj_conv(M, w, wofs, ntap, left, wcols, bcol, ppad, out, evac_out):
    P, st = M.P, M.st
    L, SL, NS = st.L, st.SL, st.NS
    if left > 0:
        P.memset(ppad[:, 0:left], 0.0)
    if ntap - 1 - left > 0:
        P.memset(ppad[:, left + L:L + ntap - 1], 0.0)
    for s in range(NS):
        ps = P.bank()
        P.mm(ps[:, 0:SL], [(w[:, k, wofs:wofs + 128], hslab(M, k, s)) for k in range(8)])
        P.act(ppad[:, left + s * SL:left + (s + 1) * SL], ps[:, 0:SL], AF.Copy, dj=(s > 0))
    dg = [M.dgring.next() for _ in range(ntap)]
    for tp in range(ntap):
        make_diag(M, dg[tp], wcols[tp])
    for s in range(NS):
        ps = P.bank()
        P.mm(ps[:, 0:SL], [(dg[tp], ppad[:, s * SL + tp:s * SL + tp + SL]) for tp in range(ntap)])
        evac_out(out[:, s * SL:(s + 1) * SL], ps[:, 0:SL], s)


def hyena_path(M):
    P, st, C, B, l = M.P, M.st, M.C, M.B, M.l
    L, NT, SL, NS = st.L, st.NT, st.SL, st.NS
    NF = NT
    m0 = P.mark()
    cw = load_cols(B, "hy_conv_w_c%d" % l, 72)
    cb = load_cols(B, "hy_conv_b_c%d" % l, 24)
    wring = Ring(P, 2, [128, 8, CG], BF16, "hywin")
    ppad = [P.tile([128, L + 2], BF16, "hyppad%d" % i) for i in range(2)]
    uT = P.tile([128, 2, L], BF16, "hy_uT")
    uTb = [Buf("hy_uT%d" % i) for i in range(2)]
    mT = P.tile([128, 2, L], BF16, "hy_mT")
    mTb = [Buf("hy_mT%d" % i) for i in range(2)]
    vtok = P.tile([128, NT, CG], BF16, "hy_vtok")
    Y = P.tile([128, NF, 2, CG], BF16, "hy_Y")
    NFR = 3
    fring = Ring(P, NFR, [128, 2 * L], BF16, "hy_dft")
    kring = Ring(P, NFR, [128, 2, 2 * CG], F32, "hy_k")
    tq = [P.tile([128, CG], F32, "hy_t%d" % i) for i in range(4)]
    fwd_src = B.inp("c_fwd%d" % L, [NT, 128, 2, NT, 128], BF16)
    inv_src = B.inp("c_inv%d" % L, [NT, 128, 2, L], BF16)
    Kscr, Kb = M.Ks
    evi = [0]

    def conv_into(dstT, dstb, base, g):
        w = load_win(M, base + g * CG, CG, wring)
        for m in range(2):
            cj = base // 128 + g * 2 + m
            wcols = [cw[:, tp * 24 + cj:tp * 24 + cj + 1] for tp in range(3)]
            bcol = cb[:, cj:cj + 1]
            out = T(dstT.ap[:, m, :], dstb[m])
            proj_conv(M, w, m * 128, 3, 1, wcols, bcol, ppad[m], out,
                      lambda d, p, s, bcol=bcol: P.act(d, p, AF.Identity, bias=bcol, dj=(s > 0)))

    def to_tok(srcT, srcb):
        gs = min(4, NT)
        for cq in range(NT // gs):
            ps = P.bank()
            pb = ps.v(ps.ap.bitcast(BF16))
            idn = C["ident_bf"]
            srcs = [T(srcT.ap[:, m, :], srcb[m]) for m in range(2)]

            def tr(pe, cq=cq, pb=pb):
                ins = None
                for cc in range(gs):
                    for m in range(2):
                        t0 = (cq * gs + cc) * 128
                        ins = pe.transpose(pb.ap[:, cc * CG + m * 128:cc * CG + (m + 1) * 128],
                                           srcT.ap[:, m, t0:t0 + 128], idn.ap)
                return ins
            P.op("pe", tr, srcs + [idn], [pb])
            dst = vtok.v(vtok.ap[:, cq * gs:(cq + 1) * gs, :].rearrange("p a b -> p (a b)"))
            evi[0] += 1
            P.act(dst, pb[:, 0:gs * CG], AF.Copy, dj=(cq > 0))

    def fft_conv(g, o, mulT, mulb, dst_fn):
        fws, kts, ivs = {}, {}, {}

        def pref_f(i):
            if 0 <= i < NF:
                fw = fring.next()
                P.dma("sp", fw, fwd_src.v(fwd_src.ap[i].rearrange("p a c j -> p (a c j)")))
                fws[i] = fw
                kt = kring.next()
                P.dma("sp", kt, T(Kscr.ap[g, i], Kb[g]))
                kts[i] = kt

        def pref_i(i):
            if 0 <= i < NF and i not in ivs:
                iv = fring.next()
                P.dma("sp", iv, inv_src.v(inv_src.ap[i].rearrange("p a t -> p (a t)")))
                ivs[i] = iv
        for i in range(NFR - 1):
            pref_f(i)
        for i in range(NF):
            pref_f(i + NFR - 1)
            pref_i(i + NFR - 1 - NF)
            fw = fws.pop(i)
            fwv = fw.v(fw.ap.rearrange("p (a c j) -> p a c j", a=2, c=NT))
            kt = kts.pop(i)
            pc = P.bank()
            pss = P.bank()
            P.mm(pc[:, 0:CG], [(fwv[:, 0, c, :], vtok[:, c, :]) for c in range(NT)])
            P.mm(pss[:, 0:CG], [(fwv[:, 1, c, :], vtok[:, c, :]) for c in range(NT)])
            kre = kt[:, 0, o * CG:(o + 1) * CG]
            kim = kt[:, 1, o * CG:(o + 1) * CG]
            P.tt(tq[0], pc[:, 0:CG], kre, ALU.mult)
            P.tt(tq[1], pss[:, 0:CG], kim, ALU.mult)
            P.tt(Y[:, i, 0, :], tq[0], tq[1], ALU.add, dj=True)
            P.tt(tq[2], pc[:, 0:CG], kim, ALU.mult)
            P.tt(tq[3], pss[:, 0:CG], kre, ALU.mult)
            P.tt(Y[:, i, 1, :], tq[2], tq[3], ALU.subtract, dj=True)
        acc = [[P.bank_reserve() for n in range(NS)] for m in range(2)]
        for i in range(NF):
            for j in range(i, i + NFR):
                pref_i(j)
            iv = ivs.pop(i)
            ivv = iv.v(iv.ap.rearrange("p (a t) -> p a t", a=2))
            for m in range(2):
                for n in range(NS):
                    P.mm_acc(acc[m][n][:, 0:SL],
                             [(Y[:, i, 0, m * 128:(m + 1) * 128], ivv[:, 0, n * SL:(n + 1) * SL]),
                              (Y[:, i, 1, m * 128:(m + 1) * 128], ivv[:, 1, n * SL:(n + 1) * SL])],
                             start=(i == 0), stop=(i == NF - 1))
        for m in range(2):
            for n in range(NS):
                P.tt(dst_fn(m, n), acc[m][n][:, 0:SL], T(mulT.ap[:, m, n * SL:(n + 1) * SL], mulb[m]), ALU.mult, dj=(n > 0))
                P.bank_free(acc[m][n])

    for g in range(D // CG):
        conv_into(uT, uTb, 2 * D, g)
        to_tok(uT, uTb)
        conv_into(mT, mTb, 0, g)
        if g == 0:
            B.dbg("hy_vT_%s%d" % (st.name, l), T(uT.ap, uTb))
        fft_conv(g, 0, mT, mTb, lambda m, n: T(uT.ap[:, m, n * SL:(n + 1) * SL], uTb[m]))
        to_tok(uT, uTb)
        conv_into(mT, mTb, D, g)
        fft_conv(g, 1, mT, mTb, lambda m, n, g=g: T(M.yT.ap[:, g * 2 + m, n * SL:(n + 1) * SL], M.yTb[g * 2 + m]))
    P.release(m0)


def lru_path(M, only_states=False):
    P, st, C, B, l = M.P, M.st, M.C, M.B, M.l
    L, NT, SL, NS = st.L, st.NT, st.SL, st.NS
    m0 = P.mark()
    cw = load_cols(B, "lru_conv_w_c%d" % l, 32)
    cb = load_cols(B, "lru_conv_b_c%d" % l, 8)
    ba = load_cols(B, "lru_ba_c%d" % l, 16)
    bx = load_cols(B, "lru_bx_c%d" % l, 16)
    lam = load_cols(B, "lru_lambda_c%d" % l, 16)
    nsp = P.tile([128, 16], F32, "lru_nsp")
    P.act(nsp, lam, AF.Exp, scale=-1.0)
    P.act(nsp, nsp, AF.Ln, bias=C["one"])
    P.ts(nsp, nsp, -8.0, None, ALU.mult)
    wring = Ring(P, 2, [128, 8, 128], BF16, "lruwin")
    gring = Ring(P, 1, [128, 8, 128], BF16, "lruwing")
    bdring = Ring(P, 8, [128, 128], BF16, "lrubd")
    for t_ in bdring.tiles:
        P.memset(t_, 0.0)
    wa_src = B.inp("lru_wa", [DEPTH, 2, 16, 64, 64], F32)
    wx_src = B.inp("lru_wx", [DEPTH, 2, 16, 64, 64], F32)
    ppad = P.tile([128, L + 3], BF16, "lruppad")
    xcf = P.tile([128, L], F32, "lru_xcf")
    xcb = P.tile([128, L], BF16, "lru_xcb")
    rr = [P.tile([128, L], F32, "lru_r%d" % d_) for d_ in range(2)]
    ii = [P.tile([128, L], F32, "lru_i%d" % d_) for d_ in range(2)]
    aa = [P.tile([128, L], F32, "lru_a%d" % d_) for d_ in range(2)]
    bb = [P.tile([128, L], F32, "lru_b%d" % d_) for d_ in range(2)]
    a_, b_ = aa[0], bb[0]
    hh = [P.tile([128, L], F32, "lru_h%d" % d_) for d_ in range(2)]
    for k in range(8):
        w = load_win(M, OFF_LRU_REC + k * 128, 128, wring)
        wcols = [cw[:, tp * 8 + k:tp * 8 + k + 1] for tp in range(4)]
        bcol = cb[:, k:k + 1]
        proj_conv(M, w, 0, 4, 2, wcols, bcol, ppad, xcf,
                  lambda d, p, s, bcol=bcol: P.act(d, p, AF.Identity, bias=bcol, dj=(s > 0)))
        P.copy(xcb, xcf, E=PE_ALT)
        import os
        CUT = int(os.environ.get("LRU_CUT", 99))
        if CUT == 1:
            break
        for dr in range(2):
            r_, i_, a_, b_ = rr[dr], ii[dr], aa[dr], bb[dr]
            bds = []
            for (src, nm) in ((wa_src, "a"), (wx_src, "x")):
                bd = bdring.next()
                for h in range(2):
                    P.dma("pool", bd[h * 64:(h + 1) * 64, h * 64:(h + 1) * 64], src.v(src.ap[l, dr, 2 * k + h]), disjoint=(h == 1))
                bds.append(bd)
            for s in range(NS):
                pa = P.bank()
                P.mm(pa[:, 0:SL], [(bds[0], xcb[:, s * SL:(s + 1) * SL])])
                P.act(r_[:, s * SL:(s + 1) * SL], pa[:, 0:SL], AF.Sigmoid, bias=ba[:, dr * 8 + k:dr * 8 + k + 1], dj=(s > 0))
                px = P.bank()
                P.mm(px[:, 0:SL], [(bds[1], xcb[:, s * SL:(s + 1) * SL])])
                P.act(i_[:, s * SL:(s + 1) * SL], px[:, 0:SL], AF.Sigmoid, bias=bx[:, dr * 8 + k:dr * 8 + k + 1], dj=(s > 0))
            if CUT == 2:
                break
            P.act(a_, r_, AF.Exp, scale=nsp[:, dr * 8 + k:dr * 8 + k + 1])
            P.act(r_, a_, AF.Square)
            P.act(r_, r_, AF.Sqrt, bias=C["one"], scale=-1.0)
            P.tt(b_, r_, i_, ALU.mult, E=PE_ALT)
            P.tt(b_, b_, xcf, ALU.mult, E=PE_ALT)
            if CUT == 3:
                break
            h_ = hh[dr]
            if st.name == "ctx":
                init = 0.0
            else:
                init = (M.stF if dr == 0 else M.stB)[:, k:k + 1]
            if dr == 0:
                P.scan(h_, a_, b_, init)
                if st.name == "ctx":
                    P.copy(M.stF[:, k:k + 1], h_[:, L - 1:L])
            else:
                P.scan(h_.v(h_.ap[:, ::-1]), a_.v(a_.ap[:, ::-1]), b_.v(b_.ap[:, ::-1]), init)
                if st.name == "ctx":
                    P.copy(M.stB[:, k:k + 1], h_[:, 0:1])
        if CUT <= 4:
            break
        if k == 0:
            B.dbg("lru_hf_%s%d" % (st.name, l), hh[0])
            B.dbg("lru_hb_%s%d" % (st.name, l), hh[1])
        if only_states:
            continue
        P.tt(hh[0], hh[0], hh[1], ALU.add, E=PE_ALT)
        wg = load_win(M, OFF_LRU_GATE + k * 128, 128, gring)
        a_, b_ = aa[0], bb[0]
        gx = a_
        for s in range(NS):
            pg = P.bank()
            P.mm(pg[:, 0:SL], [(wg[:, kk, 0:128], hslab(M, kk, s)) for kk in range(8)])
            P.act(gx[:, s * SL:(s + 1) * SL], pg[:, 0:SL], AF.Copy, dj=(s > 0))
        if CUT == 6:
            break
        P.act(b_, gx, AF.Square)
        P.ts(b_, b_, 0.044715, 1.0, ALU.mult, ALU.add, E=PE_ALT)
        P.tt(b_, b_, gx, ALU.mult, E=PE_ALT)
        P.act(b_, b_, AF.Sigmoid, scale=1.5957691216057308)
        P.tt(b_, b_, gx, ALU.mult, E=PE_ALT)
        P.tt(T(M.yT.ap[:, k, :], M.yTb[k]), b_, hh[0], ALU.mult)
        if CUT == 5 or (CUT >= 10 and k == CUT - 10):
            break
    P.release(m0)


def sconv_path(M):
    P, st, C, B, l = M.P, M.st, M.C, M.B, M.l
    L, NT, SL, NS = st.L, st.NT, st.SL, st.NS
    m0 = P.mark()
    cw = load_cols(B, "sc_conv_w_c%d" % l, 24)
    wring = Ring(P, 4, [128, 8, 128], BF16, "scwin")
    bgs = [P.tile([128, L], BF16, "sc_bg%d" % i) for i in range(2)]
    cgs = [P.tile([128, L], F32, "sc_cg%d" % i) for i in range(2)]
    ppads = [P.tile([128, L + 2], BF16, "sc_ppad%d" % i) for i in range(2)]
    for ppad in ppads:
        P.memset(ppad[:, 0:1], 0.0)
        P.memset(ppad[:, L + 1:L + 2], 0.0)
    for k in range(8):
        bg, cg, ppad = bgs[k % 2], cgs[k % 2], ppads[k % 2]
        wb = load_win(M, OFF_SC + k * 128, 128, wring)
        wc = load_win(M, OFF_SC + D + k * 128, 128, wring)
        wx = load_win(M, OFF_SC + 2 * D + k * 128, 128, wring)
        for s in range(NS):
            sl = slice(s * SL, (s + 1) * SL)
            p1 = P.bank()
            P.mm(p1[:, 0:SL], [(wb[:, kk, 0:128], hslab(M, kk, s)) for kk in range(8)])
            P.act(bg[:, sl], p1[:, 0:SL], AF.Copy, dj=(s > 0))
            p2 = P.bank()
            P.mm(p2[:, 0:SL], [(wc[:, kk, 0:128], hslab(M, kk, s)) for kk in range(8)])
            P.act(cg[:, sl], p2[:, 0:SL], AF.Copy, dj=(s > 0))
            p3 = P.bank()
            P.mm(p3[:, 0:SL], [(wx[:, kk, 0:128], hslab(M, kk, s)) for kk in range(8)])
            P.tt(ppad[:, 1 + s * SL:1 + (s + 1) * SL], p3[:, 0:SL], cg[:, sl], ALU.mult, dj=(s > 0))
        dg = [M.dgring.next() for _ in range(3)]
        for tp in range(3):
            make_diag(M, dg[tp], cw[:, tp * 8 + k:tp * 8 + k + 1])
        for s in range(NS):
            pq = P.bank()
            P.mm(pq[:, 0:SL], [(dg[tp], ppad[:, s * SL + tp:s * SL + tp + SL]) for tp in range(3)])
            P.tt(T(M.yT.ap[:, k, s * SL:(s + 1) * SL], M.yTb[k]), pq[:, 0:SL], bg[:, s * SL:(s + 1) * SL], ALU.mult, dj=(s > 0))
    P.release(m0)


def merge_path(M, pi, wname):
    P, st, C, B, l = M.P, M.st, M.C, M.B, M.l
    L, NT, SL, NS = st.L, st.NT, st.SL, st.NS
    m0 = P.mark()
    oring = Ring(P, 2, [128, 8, 128], BF16, "mg_wo")
    gring = Ring(P, 2, [128, 8, 128], BF16, "mg_wg")
    sg = [P.tile([128, SL], F32, "mg_sg%d" % i) for i in range(2)]
    tmp = [P.tile([128, SL], BF16, "mg_tmp%d" % i) for i in range(2)]
    wsrc = B.inp(wname, [DEPTH, D, D], F32)
    it = 0
    for j in range(8):
        wo = oring.next()
        P.dma("pool", wo, wsrc.v(wsrc.ap[l, :, j * 128:(j + 1) * 128].rearrange("(k p) j -> p k j", p=128)))
        wg = load_win(M, OFF_GATE + pi * D + j * 128, 128, gring)
        for s in range(NS):
            p1 = P.bank()
            P.mm(p1[:, 0:SL], [(wo[:, k, :], yslab(M, k, s)) for k in range(8)])
            p2 = P.bank()
            P.mm(p2[:, 0:SL], [(wg[:, k, 0:128], hslab(M, k, s)) for k in range(8)])
            sgi = sg[it % 2]
            tmi = tmp[it % 2]
            it += 1
            P.act(sgi, p2[:, 0:SL], AF.Sigmoid)
            dst = T(M.merged.ap[:, j, s * SL:(s + 1) * SL], M.mergedb[j][s])
            if pi == 0:
                P.tt(dst, p1[:, 0:SL], sgi, ALU.mult)
            else:
                P.tt(tmi, p1[:, 0:SL], sgi, ALU.mult)
                P.tt(dst, dst, tmi, ALU.add, E=PE_ALT)
    P.release(m0)


def bcast_cols(M, dst, cols8):
    P, C = M.P, M.C
    m0 = P.mark()
    dg = P.tile([128, 8, 128], F32, "bc_diag")
    for k in range(8):
        P.ts(dg[:, k, :], C["ident_f"], cols8[:, k:k + 1], None, ALU.mult)
    for h in range(2):
        ps = P.bank()
        P.mm(ps, [(C["ones_f"], dg.v(dg.ap[:, h * 4:(h + 1) * 4, :].rearrange("p a b -> p (a b)")))])
        P.copy(dst[:, h * 512:(h + 1) * 512], ps, E="act")
    P.release(m0)


def out_residual(M, Xd, Xdb, modcol):
    P, st, C, B, l = M.P, M.st, M.C, M.B, M.l
    m0 = P.mark()
    bc = P.tile([128, D], F32, "or_bc")
    bcast_cols(M, bc, modcol)
    wo = P.tile([128, 8, D], BF16, "or_wo")
    wsrc = B.inp("w_o", [DEPTH, D, D], F32)
    for h in range(2):
        P.dma("pool", wo[:, :, h * 512:(h + 1) * 512],
              wsrc.v(wsrc.ap[l, :, h * 512:(h + 1) * 512].rearrange("(k p) j -> p k j", p=128)), disjoint=(h == 1))
    xt = [P.tile([128, D], F32, "or_x%d" % i) for i in range(2)]
    tmp = [P.tile([128, 512], F32, "or_t%d" % i) for i in range(2)]
    for c in range(st.NT):
        x = xt[c % 2]
        xd = T(Xd.ap[c * 128:(c + 1) * 128, :], Xdb[c])
        P.dma("sp", x, xd)
        sl = (c * 128) // st.SL
        for h in range(2):
            po = P.bank()
            P.mm(po, [(T(M.merged.ap[:, k, c * 128:(c + 1) * 128], M.mergedb[k][sl]), wo[:, k, h * 512:(h + 1) * 512]) for k in range(8)])
            P.tt(tmp[h], po, bc[:, h * 512:(h + 1) * 512], ALU.mult)
            P.tt(x[:, h * 512:(h + 1) * 512], x[:, h * 512:(h + 1) * 512], tmp[h], ALU.add, E=PE_ALT)
        P.dma("sp", xd, x)
    P.release(m0)


class MoeStream:
    pass


def moe_prepare(B, C, l, st, Xd, Xdb, modT, sidx, g2, wr):
    P = B.P
    L, NT, SL, NS = st.L, st.NT, st.SL, st.NS
    cap = 2 * L // NE
    S = MoeStream()
    S.st, S.cap = st, cap
    S.nsc = max(1, cap // 128)
    S.slots = min(cap, 128)
    nm = st.name
    S.X = P.tile([128, NT, D], F32, "moeX" + nm)
    S.Xb = [Buf("moeX%s%d" % (nm, c), "moeX" + nm) for c in range(NT)]
    S.h2b = P.tile([128, NT, D], BF16, "moeh2" + nm)
    S.afft = P.tile([128, NT, NE], F32, "moeaff" + nm)
    S.affhl = P.tile([128, NT, NE, 2], BF16, "moeaffhl" + nm)
    S.post = P.tile([128, NT, NE], F32, "moepos" + nm)
    S.posb = P.tile([16, L], BF16, "moeposb" + nm)
    S.bc5 = P.tile([128, D], F32, "moebc5" + nm)
    M = MixCtx()
    M.P, M.C = P, C
    bcast_cols(M, S.bc5, modT.v(modT.ap[:, 40:48, sidx]))
    m0 = P.mark()
    geff = P.tile([128, 8], F32, "moegeff")
    P.stt(geff, modT.v(modT.ap[:, 32:40, sidx]), 1.0, g2, ALU.add, ALU.mult)
    gbc = P.tile([128, D], F32, "moegbc")
    sbc = P.tile([128, D], F32, "moesbc")
    bcast_cols(M, gbc, geff)
    bcast_cols(M, sbc, modT.v(modT.ap[:, 24:32, sidx]))
    junk = P.tile([128, D], F32, "moejunk")
    NB3 = 3
    h2f = [P.tile([128, D], F32, "moeh2f%d" % i) for i in range(NB3)]
    h2T = [P.tile([128, 8, 128], F32, "moeh2T%d" % i) for i in range(NB3)]
    ssA = P.tile([128, NT], F32, "moessA")
    rsA = P.tile([128, NT], F32, "moersA")
    mxA = P.tile([128, NT], F32, "moemxA")
    smA = P.tile([128, NT], F32, "moesmA")
    ex3 = P.tile([128, NT, NE], F32, "moeex3")
    idf = C["ident_f"]
    for c in range(NT):
        xc = T(S.X.ap[:, c, :], S.Xb[c])
        P.dma("sp", xc, T(Xd.ap[c * 128:(c + 1) * 128, :], Xdb[c]))
    for c in range(NT):
        xc = T(S.X.ap[:, c, :], S.Xb[c])
        P.act(junk, xc, AF.Square, accum=ssA[:, c:c + 1], dj=(c > 0))
    P.act(rsA, ssA, AF.Sqrt, bias=C["eps"], scale=1.0 / D)
    P.recip(rsA, rsA)
    plall = P.bank_reserve()
    for c in range(NT):
        i = c % NB3
        xc = T(S.X.ap[:, c, :], S.Xb[c])
        P.stt(h2f[i], xc, rsA[:, c:c + 1], gbc, ALU.mult, ALU.mult)
        P.tt(h2f[i], h2f[i], sbc, ALU.add)
        P.act(S.h2b[:, c, :], h2f[i], AF.Copy, dj=(c > 0))
        for hh_ in range(2):
            ps = P.bank()
            h2fi = h2f[i]

            def tr(pe, ps=ps, hh_=hh_, h2fi=h2fi):
                ins = None
                for kk in range(4):
                    k = hh_ * 4 + kk
                    ins = pe.transpose(ps.ap[:, kk * 128:(kk + 1) * 128], h2fi.ap[:, k * 128:(k + 1) * 128], idf.ap)
                return ins
            P.op("pe", tr, [h2fi, idf], [ps])
            P.act(h2T[i].v(h2T[i].ap[:, hh_ * 4:(hh_ + 1) * 4, :].rearrange("p a b -> p (a b)")), ps, AF.Copy, dj=(hh_ > 0))
        terms = [(h2T[i][:, k, :], wr[:, k, :]) for k in range(8)]
        pl = plall[:, c * NE:(c + 1) * NE]

        def rm(pe, pl=pl, terms=terms):
            ins = None
            for q, (lt, rt) in enumerate(terms):
                ins = pe.matmul(pl.ap, lt.ap, rt.ap, start=(q == 0), stop=(q == 7))
            return ins
        P.op("pe", rm, [t_ for pr_ in terms for t_ in pr_], [pl], dj=(c > 0))
    plv = plall.v(plall.ap[:, 0:NT * NE].rearrange("p (a b) -> p a b", b=NE))
    P.op("dve", lambda e: e.tensor_reduce(mxA.ap, plv.ap, mybir.AxisListType.X, ALU.max), [plv], [mxA])
    P.tt(ex3, plv, mxA.v(mxA.ap.unsqueeze(2).broadcast_to([128, NT, NE])), ALU.subtract)
    P.bank_free(plall)
    P.act(ex3, ex3, AF.Exp)
    P.op("dve", lambda e: e.tensor_reduce(smA.ap, ex3.ap, mybir.AxisListType.X, ALU.add), [ex3], [smA])
    P.recip(smA, smA)
    P.tt(S.afft, ex3, smA.v(smA.ap.unsqueeze(2).broadcast_to([128, NT, NE])), ALU.mult)
    P.release(m0)
    B.dbg("aff_%s%d" % (nm, l), S.afft)
    m0 = P.mark()
    tmpf = P.tile([128, NT, NE], F32, "moetmpf")
    P.copy(S.affhl.v(S.affhl.ap[:, :, :, 0]), S.afft)
    P.copy(tmpf, S.affhl.v(S.affhl.ap[:, :, :, 0]))
    P.tt(tmpf, S.afft, tmpf, ALU.subtract)
    P.copy(S.affhl.v(S.affhl.ap[:, :, :, 1]), tmpf)
    affT = P.tile([16, L], F32, "moeaffT")
    work = P.tile([16, L], F32, "moework")
    ones = P.tile([16, L], F32, "moeones")
    P.memset(ones, 1.0)
    gs = min(4, NT)
    for cq in range(NT // gs):
        ps = P.bank()

        def tr2(pe, ps=ps, cq=cq):
            ins = None
            for cc in range(gs):
                c = cq * gs + cc
                ins = pe.transpose(ps.ap[0:16, cc * 128:(cc + 1) * 128], S.afft.ap[:, c, :], idf.ap)
            return ins
        P.op("pe", tr2, [S.afft, idf], [ps])
        P.copy(affT[:, cq * gs * 128:(cq + 1) * gs * 128], ps[0:16, 0:gs * 128], E="act")
    P.copy(work, affT)
    m8 = P.tile([16, 8], F32, "moem8")
    for r in range(cap // 8):
        P.op("dve", lambda e: e.max(m8.ap, work.ap), [work], [m8])
        if r < cap // 8 - 1:
            P.op("dve", lambda e: e.match_replace(work.ap, m8.ap, work.ap, -1.0), [work, m8], [work])
    mask = work
    P.ts(mask, affT, m8[:, 7:8], None, ALU.is_ge)
    incl = P.tile([16, L], F32, "moeincl")
    P.scan(incl, ones, mask, 0.0)
    P.tt(incl, incl, mask, ALU.mult)
    P.ts(incl, incl, -1.0, None, ALU.add)
    P.copy(S.posb, incl)
    B.dbg("posm_%s%d" % (nm, l), incl)
    for cq in range(NT // gs):
        ps = P.bank()

        def tr3(pe, ps=ps, cq=cq):
            ins = None
            for cc in range(gs):
                c = cq * gs + cc
                ins = pe.transpose(ps.ap[:, cc * NE:(cc + 1) * NE], incl.ap[0:16, c * 128:(c + 1) * 128], idf.ap[0:16, 0:16])
            return ins
        P.op("pe", tr3, [incl, idf], [ps])
        P.copy(S.post.v(S.post.ap[:, cq * gs:(cq + 1) * gs, :].rearrange("p a b -> p (a b)")), ps[:, 0:gs * NE], E="act")
    P.release(m0)
    return S


def moe_experts(B, C, l, streams):
    P = B.P
    m0 = P.mark()
    iota = P.tile([128, 256], F32, "moeiota", sg="cols")
    P.dma("sp", iota, B.inp("c_iota", [128, 256], F32))
    pidx = P.tile([128, 2], F32, "moepidx", sg="cols")
    P.dma("sp", pidx, B.inp("c_pidx2", [128, 2], F32))
    selt = [P.tile([16, 128], BF16, "moeselt%d" % i) for i in range(2)]
    for S in streams:
        nm, st = S.st.name, S.st
        S.S = P.tile([128, st.NT, S.cap], BF16, "moeS" + nm)
        S.xsT = P.tile([128, 8, S.cap], BF16, "moexsT" + nm)
        S.hidT = P.tile([128, NFC, S.cap], BF16, "moehid" + nm)
        S.hidTb = [Buf("moehid%s%d" % (nm, i)) for i in range(NFC // 2)]
        if S.nsc == 1:
            S.yacc = P.tile([128, D], F32, "moeyacc" + nm)
        S.ysg = P.tile([128, S.nsc, D], BF16, "moeysg" + nm)
        S.gcol = P.tile([128, 2, 2], F32, "moegcol" + nm)
        S.gtmp = P.tile([128, 2], F32, "moegtmp" + nm)
        S.sg = [P.tile([128, S.cap], F32, "moesg%s%d" % (nm, i)) for i in range(2)]
    st_words = sum(S.nsc * S.st.L // 2 for S in streams)
    free = P.AW - P.top - 64 - st_words
    dbl = free - 7 * 1024 >= st_words + 64
    for S in streams:
        S.STs = [P.tile([128, S.nsc, S.st.L], BF16, "moeST%s%d" % (S.st.name, i)) for i in range(2 if dbl else 1)]
    free_slots = (P.AW - P.top - 64) // 1024
    if False:
        UPW = 512
        n_dn = 3 if free_slots < 12 else 4
        n_up = max(4, min(6, 2 * ((free_slots - n_dn) // 4)))
    else:
        UPW = 256
        n_up = max(4, min(10, 2 * int(free_slots * 0.6 / 2)))
        n_dn = max(3, min(6, free_slots - n_up))
    upring = Ring(P, n_up, [128, 8, UPW], BF16, "moeup")
    dnring = Ring(P, n_dn, [128, 2, D], BF16, "moedn")
    up_pieces = [(c0, min(UPW, DEXP - c0)) for c0 in range(0, DEXP, UPW)]
    P.peaks["moe_cfg%d" % l] = (dbl, n_up, n_dn, free_slots)
    wg_src = B.inp("w_exp_gate", [DEPTH, NE, D, DEXP], F32)
    wu_src = B.inp("w_exp_up", [DEPTH, NE, D, DEXP], F32)
    wd_src = B.inp("w_exp_down", [DEPTH, NE, DEXP, D], F32)

    def build_S(e):
        for S in streams:
            for c in range(S.st.NT):
                P.ts(S.S[:, c, :], iota[:, 0:S.cap], S.post[:, c, e:e + 1], None, ALU.is_equal, dj=(c > 0))

    def build_gate(e):
        for S in streams:
            slots = S.slots
            for sc in range(S.nsc):
                pg = P.bank()
                P.mm(pg[0:slots, 0:2], [(S.S[:, c, sc * 128:sc * 128 + slots], S.affhl[:, c, e, :]) for c in range(S.st.NT)])
                P.copy(S.gtmp[0:slots, 0:2], pg[0:slots, 0:2])
                P.tt(S.gcol[0:slots, e % 2, sc:sc + 1], S.gtmp[0:slots, 0:1], S.gtmp[0:slots, 1:2], ALU.add)

    def build_ST(e):
        sl_ = selt[e % 2]
        P.ts(sl_, C["ones_bf"][0:16, :], C["ident_f"][0:16, e:e + 1], None, ALU.mult)
        for S in streams:
            st, slots = S.st, S.slots
            STt = S.STs[e % len(S.STs)]
            for n in range(st.NS):
                pb_ = P.bank()
                P.mm(pb_[0:slots, 0:st.SL], [(sl_[:, 0:slots], S.posb[:, n * st.SL:(n + 1) * st.SL])])
                for sc in range(S.nsc):
                    P.ts(STt[0:slots, sc, n * st.SL:(n + 1) * st.SL], pb_[0:slots, 0:st.SL], pidx[0:slots, sc:sc + 1], None, ALU.is_equal, dj=(n > 0 or sc > 0))

    build_S(0)
    build_gate(0)
    build_ST(0)
    for e in range(NE):
        for S in streams:
            st = S.st
            for k in range(8):
                px = P.bank()
                P.mm(px[:, 0:S.cap], [(S.h2b[:, c, k * 128:(k + 1) * 128], S.S[:, c, :]) for c in range(st.NT)])
                P.act(S.xsT[:, k, :], px[:, 0:S.cap], AF.Copy, dj=(k > 0))
        if e + 1 < NE:
            build_S(e + 1)
        lat_streams = [S for S in streams if S.nsc > 1]
        small_streams = [S for S in streams if S.nsc == 1]
        accs = {}
        for S in lat_streams:
            accs[id(S)] = [[P.bank_reserve() for h in range(2)] for sc in range(S.nsc)]
        wds = {}

        def down_piece(p_):
            wd = wds.pop(p_)
            for S in lat_streams:
                for sc in range(S.nsc):
                    for h in range(2):
                        P.mm_acc(accs[id(S)][sc][h][0:S.slots, :],
                                 [(T(S.hidT.ap[:, p_ * 2 + mm_, sc * 128:sc * 128 + S.slots], S.hidTb[p_]), wd[:, mm_, h * 512:(h + 1) * 512]) for mm_ in range(2)],
                                 start=(p_ == 0), stop=(p_ == NFC // 2 - 1))
            for S in small_streams:
                slots = S.slots
                for h in range(2):
                    po = P.bank()
                    P.mm(po[0:slots, :], [(T(S.hidT.ap[:, p_ * 2 + mm_, 0:slots], S.hidTb[p_]), wd[:, mm_, h * 512:(h + 1) * 512]) for mm_ in range(2)])
                    ya = S.yacc[0:slots, h * 512:(h + 1) * 512]
                    if p_ == 0:
                        P.copy(ya, po[0:slots, :])
                    else:
                        P.tt(ya, ya, po[0:slots, :], ALU.add)

        for pi_, (c0, ncol) in enumerate(up_pieces):
            wg = upring.next()
            P.dma("pool", wg[:, :, 0:ncol], wg_src.v(wg_src.ap[l, e, :, c0:c0 + ncol].rearrange("(k p) j -> p k j", p=128)))
            wu = upring.next()
            P.dma("pool", wu[:, :, 0:ncol], wu_src.v(wu_src.ap[l, e, :, c0:c0 + ncol].rearrange("(k p) j -> p k j", p=128)))
            wd = dnring.next()
            P.dma("pool", wd, wd_src.v(wd_src.ap[l, e, pi_ * 256:(pi_ + 1) * 256, :].rearrange("(m p) j -> p m j", p=128)))
            wds[pi_] = wd
            for mm_ in range(ncol // 128):
                m = c0 // 128 + mm_
                for S in streams:
                    cap = S.cap
                    pg = P.bank()
                    P.mm(pg[:, 0:cap], [(wg[:, k, mm_ * 128:(mm_ + 1) * 128], S.xsT[:, k, :]) for k in range(8)])
                    pu = P.bank()
                    P.mm(pu[:, 0:cap], [(wu[:, k, mm_ * 128:(mm_ + 1) * 128], S.xsT[:, k, :]) for k in range(8)])
                    sgi = S.sg[m % 2]
                    P.act(sgi, pg[:, 0:cap], AF.Silu)
                    P.tt(T(S.hidT.ap[:, m, :], S.hidTb[m // 2]), pu[:, 0:cap], sgi, ALU.mult, dj=(m % 2 == 1))
            if pi_ >= 1:
                down_piece(pi_ - 1)
        down_piece(NFC // 2 - 1)
        if e + 1 < NE:
            build_gate(e + 1)
            if dbl:
                build_ST(e + 1)
        for S in lat_streams:
            slots = S.slots
            for sc in range(S.nsc):
                for h in range(2):
                    a = accs[id(S)][sc][h]
                    P.stt(S.ysg[0:slots, sc, h * 512:(h + 1) * 512], a[0:slots, :], S.gcol[0:slots, e % 2, sc:sc + 1],
                          S.bc5[0:slots, h * 512:(h + 1) * 512], ALU.mult, ALU.mult)
                    P.bank_free(a)
        for S in small_streams:
            slots = S.slots
            for h in range(2):
                P.stt(S.ysg[0:slots, 0, h * 512:(h + 1) * 512], S.yacc[0:slots, h * 512:(h + 1) * 512], S.gcol[0:slots, e % 2, 0:1],
                      S.bc5[0:slots, h * 512:(h + 1) * 512], ALU.mult, ALU.mult)
        for S in streams:
            st, slots = S.st, S.slots
            for c in range(st.NT):
                for h in range(2):
                    po = P.bank()
                    STt = S.STs[e % len(S.STs)]
                    P.mm(po, [(STt[0:slots, sc, c * 128:(c + 1) * 128], S.ysg[0:slots, sc, h * 512:(h + 1) * 512]) for sc in range(S.nsc)])
                    xc = T(S.X.ap[:, c, h * 512:(h + 1) * 512], S.Xb[c])
                    P.tt(xc, xc, po, ALU.add)
        if e + 1 < NE and not dbl:
            build_ST(e + 1)
    P.release(m0)


def build_program(dbg=(), stop=None):
    B = Builder(dbg, stop)
    P = B.P
    C = build_consts(B)
    lat = Stream("lat", SEQ)
    ctx = Stream("ctx", CTXL)
    Xd = B.scratch("Xd", [SEQ, D], F32)
    Xdb = [Buf("Xd%d" % c, "Xd") for c in range(lat.NT)]
    XCd = B.scratch("XCd", [CTXL, D], F32)
    XCdb = [Buf("XCd%d" % c, "XCd") for c in range(ctx.NT)]
    xin = B.inp("x", [SEQ, D], F32)
    pos = B.inp("c_pos", [SEQ, D], F32)
    cin = B.inp("ctx", [CTXL, D], F32)
    m = P.mark()
    xtmp = [P.tile([128, D], F32, "xtmp%d" % i) for i in range(4)]
    ptmp = [P.tile([128, D], F32, "ptmp%d" % i) for i in range(4)]
    def pl(c):
        if c < lat.NT:
            P.dma("sp", xtmp[c % 4], xin.v(xin.ap[c * 128:(c + 1) * 128, :]))
            P.dma("sp", ptmp[c % 4], pos.v(pos.ap[c * 128:(c + 1) * 128, :]))
    for c in range(3):
        pl(c)
    for c in range(lat.NT):
        pl(c + 3)
        P.tt(xtmp[c % 4], xtmp[c % 4], ptmp[c % 4], ALU.add)
        P.dma("act", T(Xd.ap[c * 128:(c + 1) * 128, :], Xdb[c]), xtmp[c % 4])
    for c in range(ctx.NT):
        P.dma("sp", ptmp[c % 2], cin.v(cin.ap[c * 128:(c + 1) * 128, :]))
        P.dma("sp", T(XCd.ap[c * 128:(c + 1) * 128, :], XCdb[c]), ptmp[c % 2])
    P.release(m)
    Ks = {}
    for st in (ctx, lat):
        Ks[st.name] = (B.scratch("Kscr_" + st.name, [D // CG, st.NT, 128, 2, 2 * CG], F32), [Buf("K%s%d" % (st.name, g), "K" + st.name) for g in range(D // CG)])
    if stop is None:
        for st in (ctx, lat):
            filter_phase(B, C, 0, st, Ks[st.name][0], Ks[st.name][1])
        filt0_done = True
    else:
        filt0_done = False
    mods = adaln(B, C)
    stF = P.tile([128, 8], F32, "stF")
    stB = P.tile([128, 8], F32, "stB")
    win = B.inp("w_in", [DEPTH, D, DIN], F32)
    for l in range(DEPTH):
        last = l == DEPTH - 1
        mL = P.mark()
        for st in ((lat,) if last else (ctx, lat)):
            if l == 0 and filt0_done:
                continue
            filter_phase(B, C, l, st, Ks[st.name][0], Ks[st.name][1])
            P.peak("filter_" + st.name)
            if stop == "filter_" + st.name:
                kt = P.tile([128, 2, 2 * CG], F32, "kdbg")
                for (g_, f_) in ((0, 0), (3, st.NT - 1)):
                    P.dma("sp", kt, T(Ks[st.name][0].ap[g_, f_], Ks[st.name][1][g_]))
                    B.dbg("K_%d_%d" % (g_, f_), kt)
                return B.finish([]) or B
        g1 = load_cols(B, "norm1_g_c%d" % l, 8)
        geff = P.tile([128, 8, 2], F32, "geff1")
        for s in range(2):
            P.stt(geff.v(geff.ap[:, :, s]), mods[l].v(mods[l].ap[:, 8:16, s]), 1.0, g1, ALU.add, ALU.mult)
        for st, Xs, Xsb, sidx in ((ctx, XCd, XCdb, 1), (lat, Xd, Xdb, 0)):
            mS = P.mark()
            M = MixCtx()
            M.P, M.st, M.C, M.B, M.l, M.win = P, st, C, B, l, win
            M.Ks = Ks[st.name]
            M.stF, M.stB = stF, stB
            M.hT, M.hTb = hT_alloc(P, st, "hT")
            norm_to_hT(B, C, st, Xs, Xsb, geff.v(geff.ap[:, :, sidx]), mods[l].v(mods[l].ap[:, 0:8, sidx]), M.hT, M.hTb)
            M.dgring = Ring(P, 8, [128, 128], BF16, "diag")
            tag = "%s%d" % (st.name, l)
            if st is ctx and last:
                lru_path(M, only_states=True)
                P.release(mS)
                continue
            M.yT = P.tile([128, 8, st.L], BF16, "yT")
            M.yTb = [Buf("yT%d" % k) for k in range(8)]
            M.merged = P.tile([128, 8, st.L], BF16, "merged")
            M.mergedb = [[Buf("mg%d_%d" % (k, s)) for s in range(st.NS)] for k in range(8)]
            yTall = T(M.yT.ap, M.yTb)
            P.peak("pre")
            hyena_path(M)
            P.peak("hyena_" + st.name)
            B.dbg("ya_" + tag, yTall)
            if stop == "hyena_" + tag:
                return B.finish([]) or B
            merge_path(M, 0, "w_hy_out")
            if stop == "merge0_" + tag:
                B.dbg("merged_" + tag, T(M.merged.ap, [b for r in M.mergedb for b in r]))
                return B.finish([]) or B
            lru_path(M)
            P.peak("lru_" + st.name)
            B.dbg("yb_" + tag, yTall)
            if stop == "lru_" + tag:
                return B.finish([]) or B
            merge_path(M, 1, "w_lru_out")
            sconv_path(M)
            B.dbg("yc_" + tag, yTall)
            merge_path(M, 2, "w_sc_out")
            B.dbg("merged_" + tag, T(M.merged.ap, [b for r in M.mergedb for b in r]))
            if stop == "merged_" + tag:
                return B.finish([]) or B
            out_residual(M, Xs, Xsb, mods[l].v(mods[l].ap[:, 16:24, sidx]))
            if stop == "mix_" + tag:
                xo = P.tile([128, st.NT, D], F32, "xdbg")
                for c in range(st.NT):
                    P.dma("sp", xo[:, c, :], T(Xs.ap[c * 128:(c + 1) * 128, :], Xsb[c]), disjoint=(c > 0))
                B.dbg("xmix_" + tag, xo)
                return B.finish([]) or B
            P.release(mS)
        g2 = load_cols(B, "norm2_g_c%d" % l, 8)
        wr = P.tile([128, 8, NE], F32, "wrouter")
        wrs = B.inp("w_router", [DEPTH, D, NE], F32)
        P.dma("sp", wr, wrs.v(wrs.ap[l].rearrange("(k p) e -> p k e", p=128)))
        streams = []
        if not last:
            streams.append(moe_prepare(B, C, l, ctx, XCd, XCdb, mods[l], 1, g2, wr))
        streams.append(moe_prepare(B, C, l, lat, Xd, Xdb, mods[l], 0, g2, wr))
        if stop == "moeprep%d" % l:
            return B.finish([]) or B
        P.peak("moeprep%d" % l)
        moe_experts(B, C, l, streams)
        P.peak("moe%d" % l)
        for S in streams:
            st = S.st
            Xs, Xsb = (XCd, XCdb) if st is ctx else (Xd, Xdb)
            B.dbg("xmoe_%s%d" % (st.name, l), T(S.X.ap, S.Xb))
            if st is lat and last:
                break
            for c in range(st.NT):
                P.dma("sp", T(Xs.ap[c * 128:(c + 1) * 128, :], Xsb[c]), T(S.X.ap[:, c, :], S.Xb[c]))
        if stop == "moe%d" % l:
            return B.finish([]) or B
        if last:
            S = streams[-1]
            out = B.nc.dram_tensor("out", [SEQ, D], F32, kind="ExternalOutput").ap()
            outT = T(out, Buf("out"))
            gf = load_cols(B, "final_norm_g_cx", 8)
            gfbc = P.tile([128, D], F32, "gfbc")
            Mx = MixCtx()
            Mx.P, Mx.C = P, C
            bcast_cols(Mx, gfbc, gf)
            junk = P.tile([128, D], F32, "fjunk")
            ot = [P.tile([128, D], F32, "fo%d" % i) for i in range(2)]
            ss = [P.tile([128, 1], F32, "fss%d" % i) for i in range(2)]
            for c in range(lat.NT):
                i = c % 2
                xc = T(S.X.ap[:, c, :], S.Xb[c])
                P.act(junk, xc, AF.Square, accum=ss[i])
                P.act(ss[i], ss[i], AF.Sqrt, bias=C["eps"], scale=1.0 / D)
                P.recip(ss[i], ss[i])
                P.stt(ot[i], xc, ss[i], gfbc, ALU.mult, ALU.mult)
                P.dma("sp", outT.v(out[c * 128:(c + 1) * 128, :]), ot[i], disjoint=(c > 0))
            B.finish([outT])
            return B
        P.release(mL)
    B.finish([])
    return B


_CONST_CACHE = {}


def _const(name):
    if name in _CONST_CACHE:
        return _CONST_CACHE[name]
    if name == "c_ident_bf":
        v = bf(np.eye(128))
    elif name == "c_ident_f":
        v = np.eye(128, dtype=np.float32)
    elif name == "c_pos":
        v = grid_pos_embed(SEQ // 64)
    elif name.startswith("c_fwd") or name.startswith("c_inv"):
        L = int(name[5:])
        F, I = dft_consts(L)
        _CONST_CACHE["c_fwd%d" % L] = F
        _CONST_CACHE["c_inv%d" % L] = I
        return _CONST_CACHE[name]
    elif name.startswith("c_feats") or name.startswith("c_decay"):
        L = int(name[7:])
        fT, dec = filt_consts(L)
        _CONST_CACHE["c_feats%d" % L] = fT
        _CONST_CACHE["c_decay%d" % L] = dec
        return _CONST_CACHE[name]
    elif name == "c_iota":
        v = np.broadcast_to(np.arange(256, dtype=np.float32)[None, :], (128, 256)).copy()
    elif name == "c_pidx2":
        v = np.stack([np.arange(128, dtype=np.float32), np.arange(128, dtype=np.float32) + 128], axis=1).copy()
    elif name == "c_sel":
        v = np.zeros((16, NE, 128), np.float32)
        for e_ in range(NE):
            v[e_, e_, :] = 1.0
        v = bf(v)
    elif name == "c_pidx":
        v = np.arange(128, dtype=np.float32).reshape(128, 1).copy()
    else:
        raise KeyError(name)
    _CONST_CACHE[name] = v
    return v


def host_arrays(inputs, b, needed):
    out = {}
    for name in needed:
        if name.startswith("c_"):
            out[name] = _const(name)
        elif name == "x":
            out[name] = np.ascontiguousarray(inputs["x"][b])
        elif name == "ctx":
            out[name] = np.ascontiguousarray(inputs["ctx"][b])
        elif name == "cvec":
            out[name] = np.concatenate([col(inputs["c"][b]), col(inputs["c_ctx"])], axis=1)
        elif name in inputs:
            out[name] = np.ascontiguousarray(inputs[name])
        elif name == "final_norm_g_cx":
            out[name] = col(inputs["final_norm_g"])
        elif name.startswith("filtcols_"):
            l = int(name.split("_")[1])
            out[name] = np.ascontiguousarray(np.stack([inputs["hy_filt_b1"][l], inputs["hy_filt_b2"][l], inputs["hy_filt_freq"][l]], axis=1).astype(np.float32))
        else:
            base, l = name.rsplit("_c", 1)
            l = int(l)
            colsrc = {"b_ada": inputs["b_ada"], "norm1_g": inputs["norm1_g"], "norm2_g": inputs["norm2_g"],
                      "hy_conv_w": inputs["hy_conv_w"], "hy_conv_b": inputs["hy_conv_b"],
                      "lru_conv_w": inputs["lru_conv_w"], "lru_conv_b": inputs["lru_conv_b"],
                      "lru_ba": inputs["lru_ba"], "lru_bx": inputs["lru_bx"], "lru_lambda": inputs["lru_lambda"],
                      "sc_conv_w": inputs["sc_conv_w"]}
            out[name] = col(np.asarray(colsrc[base][l]).reshape(-1))
    return out


TWO_PI = 2.0 * math.pi


def sin_reduced(P, out, arg, tmp_i, tmp_f):
    P.ts(tmp_f, arg, 1.0 / TWO_PI, None, ALU.mult)
    P.copy(tmp_i, tmp_f)
    P.copy(tmp_f, tmp_i)
    P.stt(arg, tmp_f, -TWO_PI, arg, ALU.mult, ALU.add)
    P.ts(tmp_f, arg, math.pi, TWO_PI, ALU.is_gt, ALU.mult)
    P.tt(arg, arg, tmp_f, ALU.subtract)
    P.ts(tmp_f, arg, -math.pi, TWO_PI, ALU.is_lt, ALU.mult)
    P.tt(arg, arg, tmp_f, ALU.add)
    P.ts(arg, arg, math.pi, -math.pi, ALU.min, ALU.max)
    P.act(out, arg, AF.Sin)


def filter_phase(B, C, l, st, Kscr, Kb):
    P = B.P
    L, NT = st.L, st.NT
    SL, NS = st.SL, st.NS
    m0 = P.mark()
    h2 = P.tile([64, L], F32, "fh2")
    w3 = P.tile([64, 4096], F32, "fw3")
    P.dma("sp", w3, B.inp("hy_filt_w3", [DEPTH, 64, 4096]).v(B.inp("hy_filt_w3", [DEPTH, 64, 4096]).ap[l]))
    mt = P.mark()
    featsT = P.tile([33, L], F32, "featsT")
    P.dma("sp", featsT, B.inp("c_feats%d" % L, [33, L], F32))
    w1 = P.tile([33, 64], F32, "fw1")
    P.dma("sp", w1, B.inp("hy_filt_w1", [DEPTH, 33, 64]).v(B.inp("hy_filt_w1", [DEPTH, 33, 64]).ap[l]))
    w2 = P.tile([64, 64], F32, "fw2")
    P.dma("sp", w2, B.inp("hy_filt_w2", [DEPTH, 64, 64]).v(B.inp("hy_filt_w2", [DEPTH, 64, 64]).ap[l]))
    fc = P.tile([64, 3], F32, "fcols")
    P.dma("sp", fc, B.inp("filtcols_%d" % l, [64, 3], F32))
    fb = P.tile([64, 2], F32, "fb")
    P.tt(fb[:, 0:1], fc[:, 0:1], fc[:, 2:3], ALU.mult)
    P.tt(fb[:, 1:2], fc[:, 1:2], fc[:, 2:3], ALU.mult)
    arg = P.tile([64, L], F32, "farg")
    tf = P.tile([64, L], F32, "ftf")
    ti = P.tile([64, L], I32, "fti")
    h1 = P.tile([64, L], F32, "fh1")
    for (wmat, src, dst, bi) in ((w1, featsT, h1, 0), (w2, h1, h2, 1)):
        for s_ in range(NS):
            ps = P.bank()
            pv = ps[0:64, 0:SL]
            P.mm(pv, [(wmat, src[:, s_ * SL:(s_ + 1) * SL])])
            P.act(arg[:, s_ * SL:(s_ + 1) * SL], pv, AF.Identity, bias=fb[:, bi:bi + 1], scale=fc[:, 2:3])
        sin_reduced(P, dst, arg, ti, tf)
    P.release(mt)
    B.dbg("filt_h2_%s%d" % (st.name, l), h2)
    fwd_src = B.inp("c_fwd%d" % L, [NT, 128, 2, NT, 128], BF16)
    dec_src = B.inp("c_decay%d" % L, [128, NT, D], F32)
    hb_src = B.inp("hy_bias", [DEPTH, 2, D], F32)
    h2b = P.tile([64, L], BF16, "fh2b")
    P.copy(h2b, h2)
    w3b = P.tile([64, 4096], BF16, "fw3b")
    P.copy(w3b, w3, E="act")
    m1 = P.mark()
    NG = D // CG
    NFR = 4
    fring = [P.tile([128, 2, NT, 128], BF16, "ffwd%d" % i) for i in range(NFR)]
    kout = [P.tile([128, 2, 2 * CG], F32, "fkout%d" % i) for i in range(3)]
    hd = [P.tile([128, 2, 2, CG], F32, "fhd%d" % i) for i in range(3)]
    ab = [P.tile([128, 2, 2, CG], BF16, "fab%d" % i) for i in range(3)]
    G = []
    for pp in range(2):
        gs_ = MixCtx()
        gs_.dec = P.tile([128, NT, CG], F32, "fdec%d" % pp)
        gs_.Eg = P.tile([128, NT, 2 * CG], BF16, "fE%d" % pp)
        gs_.Og = P.tile([128, NT, 2 * CG], BF16, "fO%d" % pp)
        gs_.biasbc = P.tile([128, 2 * CG], F32, "fbias%d" % pp)
        gs_.rnorm = P.tile([128, 2 * CG], F32, "frnorm%d" % pp)
        G.append(gs_)

    def g_begin(g):
        gs_ = G[g % 2]
        P.dma("sp", gs_.dec, dec_src.v(dec_src.ap[:, :, g * CG:(g + 1) * CG]))
        for o in range(2):
            P.dma("sp", gs_.biasbc[:, o * CG:(o + 1) * CG],
                  hb_src.v(hb_src.ap[l, o:o + 1, g * CG:(g + 1) * CG].partition_broadcast(128)), disjoint=(o == 1))
        gs_.nps = P.bank_reserve()

    def g_stage1(g, c):
        gs_ = G[g % 2]
        i = c % 3
        for o in range(2):
            ps = P.bank()
            for dr in range(2):
                colo = (o * 2 + dr) * D + g * CG
                P.mm(ps[:, dr * CG:(dr + 1) * CG], [(h2b[:, c * 128:(c + 1) * 128], w3b[:, colo:colo + CG])])
            decb = gs_.dec.v(gs_.dec.ap[:, c, :].unsqueeze(1).broadcast_to([128, 2, CG]))
            P.tt(hd[i][:, o, :, :], ps.v(ps.ap.rearrange("p (a b) -> p a b", a=2)), decb, ALU.mult, dj=(o > 0))
        if c == 0:
            P.memset(hd[i][0:1, :, 1, :], 0.0)
        P.act(ab[i], hd[i], AF.Abs)
        Ev = gs_.Eg.v(gs_.Eg.ap[:, c, :].rearrange("p (a b) -> p a b", a=2))
        Ov = gs_.Og.v(gs_.Og.ap[:, c, :].rearrange("p (a b) -> p a b", a=2))
        P.tt(Ev, hd[i][:, :, 0, :], hd[i][:, :, 1, :], ALU.add, dj=(c > 0))
        P.tt(Ov, hd[i][:, :, 1, :], hd[i][:, :, 0, :], ALU.subtract, dj=(c > 0))

    def g_stage2(g, c):
        gs_ = G[g % 2]
        i = c % 3
        for o in range(2):
            P.mm_acc(gs_.nps[:, o * CG:(o + 1) * CG],
                     [(C["ones_bf"], ab[i][:, o, 0, :]), (C["ones_bf"], ab[i][:, o, 1, :])],
                     start=(c == 0 and o == 0), stop=(c == NT - 1 and o == 1))

    def g_end(g):
        gs_ = G[g % 2]
        P.recip(gs_.rnorm, gs_.nps)
        P.bank_free(gs_.nps)
        if g == 0:
            B.dbg("filt_rnorm_%s%d" % (st.name, l), gs_.rnorm)

    def pref(g, fi):
        if g < NG and fi < NT:
            P.dma("sp", fring[(g * NT + fi) % NFR], fwd_src.v(fwd_src.ap[fi]))

    def g_dft(g, fi):
        gs_ = G[g % 2]
        fw = fring[(g * NT + fi) % NFR]
        pr = P.bank()
        P.mm(pr, [(fw[:, 0, c, :], gs_.Eg[:, c, :]) for c in range(NT)])
        pi_ = P.bank()
        P.mm(pi_, [(fw[:, 1, c, :], gs_.Og[:, c, :]) for c in range(NT)])
        ko = kout[fi % 3]
        P.tt(ko[:, 0, :], pr, gs_.rnorm, ALU.mult)
        P.tt(ko[:, 0, :], ko[:, 0, :], gs_.biasbc, ALU.add)
        P.tt(ko[:, 1, :], pi_, gs_.rnorm, ALU.mult)
        P.dma("act", T(Kscr.ap[g, fi], Kb[g]), ko, disjoint=True)

    g_begin(0)
    for c in range(NT):
        g_stage1(0, c)
        g_stage2(0, c)
    g_end(0)
    for g in range(NG):
        for q in range(NFR - 1):
            pref(g, q)
        if g + 1 < NG:
            g_begin(g + 1)
        for t_ in range(NT):
            pref(g, t_ + NFR - 1)
            if g + 1 < NG:
                g_stage1(g + 1, t_)
            g_dft(g, t_)
            if g + 1 < NG:
                g_stage2(g + 1, t_)
        if g + 1 < NG:
            g_end(g + 1)
    P.release(m0)


_PROGRAM = [None]


def _get_program():
    if _PROGRAM[0] is None:
        _PROGRAM[0] = build_program()
    return _PROGRAM[0]


def kernel(**inputs):
    inputs = {k: np.asarray(v) for k, v in inputs.items()}
    B = _get_program()
    names = list(B.inputs.keys())
    shared = {}
    in_maps = []
    nb = inputs["x"].shape[0]
    for b in range(nb):
        per_core = {"x", "ctx", "cvec"}
        need_b = set(n for n in names if n in per_core or n not in shared)
        h = host_arrays(inputs, b, need_b)
        for n in names:
            if n not in per_core and n not in shared:
                shared[n] = h[n]
        in_maps.append({n: (h[n] if n in per_core else shared[n]) for n in names})
    res = run_bass_kernel_spmd(B.nc, in_maps, core_ids=list(range(nb)))
    out = np.stack([np.asarray(res.results[b]["out"], dtype=np.float32) for b in range(nb)], axis=0)
    return out
```
